# Optimizing a Trainium2 kernel written in Bass

```python
import math
import jax, jax.numpy as jnp
from jax import lax
import numpy as np

D_MODEL = 1024
BATCH = 8
SEQ = 4096
DEPTH = 1

MEM_TOKENS = 256
RMS_EPS = 1e-5

D_MIX = D_MODEL
HEAD_DIM = 64
ATTN_WIDTH = D_MIX // 2
N_Q_HEADS = ATTN_WIDTH // HEAD_DIM
N_KV_HEADS = 2
Q_PER_KV = N_Q_HEADS // N_KV_HEADS
WINDOW = 128
ATTN_BLOCK = 128
ROT_DIM = HEAD_DIM // 4
ROPE_THETA = 500000.0
MAX_POS_OFFSET = 2048

SSD_WIDTH = D_MIX - ATTN_WIDTH
SSD_HEAD_DIM = 64
SSD_HEADS = SSD_WIDTH // SSD_HEAD_DIM
SSD_GROUPS = 2
SSD_HEADS_PER_GROUP = SSD_HEADS // SSD_GROUPS
SSD_STATE = 128
SSD_CONV = 4
SSD_CHUNK = 128
SSD_CONV_DIM = SSD_WIDTH + 2 * SSD_GROUPS * SSD_STATE
DT_MIN = 0.001
DT_MAX = 0.1

SPLIT_POINTS = (
    ATTN_WIDTH,
    ATTN_WIDTH + N_KV_HEADS * HEAD_DIM,
    ATTN_WIDTH + 2 * N_KV_HEADS * HEAD_DIM,
    ATTN_WIDTH + 2 * N_KV_HEADS * HEAD_DIM + SSD_WIDTH,
    ATTN_WIDTH + 2 * N_KV_HEADS * HEAD_DIM + SSD_WIDTH + SSD_CONV_DIM,
)
IN_PROJ_DIM = SPLIT_POINTS[-1] + SSD_HEADS

XATTN_HEADS = 4
XATTN_HEAD_DIM = 128
XATTN_WIDTH = XATTN_HEADS * XATTN_HEAD_DIM

N_GROUPS = 4
EXPERTS_PER_GROUP = 8
N_EXPERTS = N_GROUPS * EXPERTS_PER_GROUP
TOP_K = 2
EXPERT_FF = D_MODEL // 4

kernel_name = 'hybrid_swa_ssd_hier_moe_block'


def rms_norm(x, w):
    xf = x.astype(jnp.float32)
    y = xf * lax.rsqrt(jnp.mean(xf * xf, axis=-1, keepdims=True) + RMS_EPS)
    return (y * w.astype(jnp.float32)).astype(x.dtype)


def partial_rope(t, positions):
    half = ROT_DIM // 2
    inv_freq = jnp.float32(ROPE_THETA) ** (-jnp.arange(0, ROT_DIM, 2, dtype=jnp.float32) / ROT_DIM)
    ang = positions.astype(jnp.float32)[..., None] * inv_freq
    cos = jnp.cos(ang)[:, :, None, :]
    sin = jnp.sin(ang)[:, :, None, :]
    tf = t.astype(jnp.float32)
    x1, x2, rest = tf[..., :half], tf[..., half:ROT_DIM], tf[..., ROT_DIM:]
    out = jnp.concatenate([x1 * cos - x2 * sin, x2 * cos + x1 * sin, rest], axis=-1)
    return out.astype(t.dtype)


def sliding_window_attention(q, k, v, sinks):
    b, s = q.shape[0], q.shape[1]
    nb = s // ATTN_BLOCK
    qb = q.reshape(b, nb, ATTN_BLOCK, N_KV_HEADS, Q_PER_KV, HEAD_DIM)

    def band(t):
        tb = t.reshape(b, nb, ATTN_BLOCK, N_KV_HEADS, HEAD_DIM)
        prev = jnp.pad(tb[:, :-1], ((0, 0), (1, 0), (0, 0), (0, 0), (0, 0)))
        return jnp.concatenate([prev, tb], axis=2)

    kb, vb = band(k), band(v)
    scores = jnp.einsum('bnqhgd,bnkhd->bnhgqk', qb, kb,
                        preferred_element_type=jnp.float32) * (HEAD_DIM ** -0.5)
    q_idx = jnp.arange(ATTN_BLOCK)[:, None] + ATTN_BLOCK
    k_idx = jnp.arange(2 * ATTN_BLOCK)[None, :]
    rel = q_idx - k_idx
    local = (rel >= 0) & (rel < WINDOW)
    valid_prev = (jnp.arange(nb)[:, None, None] > 0) | (k_idx[None] >= ATTN_BLOCK)
    mask = local[None] & valid_prev
    scores = jnp.where(mask[None, :, None, None], scores, -jnp.inf)
    sink = sinks.astype(jnp.float32).reshape(N_KV_HEADS, Q_PER_KV)[None, None, :, :, None, None]
    m = jnp.maximum(jnp.max(scores, axis=-1, keepdims=True), sink)
    p = jnp.exp(scores - m)
    probs = p / (jnp.sum(p, axis=-1, keepdims=True) + jnp.exp(sink - m))
    out = jnp.einsum('bnhgqk,bnkhd->bnqhgd', probs.astype(v.dtype), vb)
    return out.reshape(b, s, N_Q_HEADS * HEAD_DIM)


def causal_depthwise_conv(t, w, bias):
    c = t.shape[-1]
    out = lax.conv_general_dilated(t, w[:, None, :], window_strides=(1,),
                                   padding=[(SSD_CONV - 1, 0)],
                                   dimension_numbers=('NWC', 'WIO', 'NWC'),
                                   feature_group_count=c)
    return out + bias


def ssd_chunked(xs, dt, a, bm, cm):
    b, s = xs.shape[0], xs.shape[1]
    nc = s // SSD_CHUNK
    G, R, P, L = SSD_GROUPS, SSD_HEADS_PER_GROUP, SSD_HEAD_DIM, SSD_CHUNK
    xc = (xs * dt[..., None]).reshape(b, nc, L, G, R, P)
    da = (dt * a).reshape(b, nc, L, G, R)
    da_cum = jnp.cumsum(da, axis=2)
    bc = bm.reshape(b, nc, L, G, SSD_STATE)
    cc = cm.reshape(b, nc, L, G, SSD_STATE)
    seg = da_cum[:, :, :, None] - da_cum[:, :, None, :]
    causal = jnp.tril(jnp.ones((L, L), dtype=bool))
    decay = jnp.exp(jnp.where(causal[:, :, None, None], seg, -jnp.inf))
    cb = jnp.einsum('bclgn,bcsgn->bclsg', cc, bc)
    y_diag = jnp.einsum('bclsgr,bcsgrp->bclgrp', cb[..., None] * decay, xc)
    decay_to_end = jnp.exp(da_cum[:, :, -1:] - da_cum)
    chunk_states = jnp.einsum('bclgn,bclgrp->bcgrpn', bc, xc * decay_to_end[..., None])
    chunk_decay = jnp.exp(da_cum[:, :, -1])

    def step(state, inp):
        st_c, dec_c = inp
        return state * dec_c[..., None, None] + st_c, state

    init = jnp.zeros((b, G, R, P, SSD_STATE), jnp.float32)
    _, start_states = lax.scan(step, init, (jnp.moveaxis(chunk_states, 1, 0),
                                            jnp.moveaxis(chunk_decay, 1, 0)))
    start_states = jnp.moveaxis(start_states, 0, 1)
    y_off = jnp.einsum('bclgn,bcgrpn->bclgrp', cc, start_states) * jnp.exp(da_cum)[..., None]
    return (y_diag + y_off).reshape(b, s, SSD_HEADS, P)


def gated_group_rms_norm(y, z, w):
    b, s, _ = y.shape
    g = (y * jax.nn.silu(z.astype(jnp.float32))).reshape(b, s, SSD_GROUPS, -1)
    g = g * lax.rsqrt(jnp.mean(g * g, axis=-1, keepdims=True) + RMS_EPS)
    return g.reshape(b, s, SSD_WIDTH) * w.astype(jnp.float32)


def hybrid_mixer(h, positions, w_in, sinks, conv_w, conv_b, dt_bias, a_log, d_skip,
                 attn_out_norm_w, ssd_out_norm_w, w_out):
    b, s, _ = h.shape
    proj = h @ w_in
    q, k, v, z, xbc, dt_raw = jnp.split(proj, SPLIT_POINTS, axis=-1)
    q = partial_rope(q.reshape(b, s, N_Q_HEADS, HEAD_DIM), positions)
    k = partial_rope(k.reshape(b, s, N_KV_HEADS, HEAD_DIM), positions)
    v = v.reshape(b, s, N_KV_HEADS, HEAD_DIM)
    attn = rms_norm(sliding_window_attention(q, k, v, sinks), attn_out_norm_w)
    xbc = jax.nn.silu(causal_depthwise_conv(xbc, conv_w, conv_b))
    xs, bm, cm = jnp.split(xbc, (SSD_WIDTH, SSD_WIDTH + SSD_GROUPS * SSD_STATE), axis=-1)
    xs = xs.reshape(b, s, SSD_HEADS, SSD_HEAD_DIM).astype(jnp.float32)
    dt = jax.nn.softplus(dt_raw.astype(jnp.float32) + dt_bias.astype(jnp.float32))
    a = -jnp.exp(a_log.astype(jnp.float32))
    y = ssd_chunked(xs, dt, a,
                    bm.reshape(b, s, SSD_GROUPS, SSD_STATE).astype(jnp.float32),
                    cm.reshape(b, s, SSD_GROUPS, SSD_STATE).astype(jnp.float32))
    y = y + d_skip.astype(jnp.float32)[:, None] * xs
    ssd = gated_group_rms_norm(y.reshape(b, s, SSD_WIDTH), z, ssd_out_norm_w).astype(h.dtype)
    return jnp.concatenate([attn, ssd], axis=-1) @ w_out


def memory_cross_attention(h, mem_h, w_q, w_kv, w_o):
    b, s, _ = h.shape
    m = mem_h.shape[1]
    q = (h @ w_q).reshape(b, s, XATTN_HEADS, XATTN_HEAD_DIM)
    kv = (mem_h @ w_kv).reshape(b, m, 2, XATTN_HEADS, XATTN_HEAD_DIM)
    k, v = kv[:, :, 0], kv[:, :, 1]
    scores = jnp.einsum('bshd,bmhd->bhsm', q, k,
                        preferred_element_type=jnp.float32) * (XATTN_HEAD_DIM ** -0.5)
    probs = jax.nn.softmax(scores, axis=-1).astype(v.dtype)
    out = jnp.einsum('bhsm,bmhd->bshd', probs, v).reshape(b, s, XATTN_WIDTH)
    return out @ w_o


def hierarchical_moe(h, wg, bg, we, be, w_gate, w_up, w_down):
    b, s, d = h.shape
    t = h.reshape(b * s, d)
    g_prob = jax.nn.softmax((t @ wg).astype(jnp.float32) + bg.astype(jnp.float32), axis=-1)
    g_gate, g_idx = lax.top_k(g_prob, 1)
    e_logits = ((t @ we).astype(jnp.float32) + be.astype(jnp.float32)).reshape(-1, N_GROUPS, EXPERTS_PER_GROUP)
    e_sel = jnp.take_along_axis(e_logits, g_idx[:, :, None], axis=1)[:, 0]
    top_vals, top_idx = lax.top_k(e_sel, TOP_K)
    top_w = jax.nn.softmax(top_vals, axis=-1) * g_gate
    expert_id = g_idx * EXPERTS_PER_GROUP + top_idx
    gates = jnp.sum(jax.nn.one_hot(expert_id, N_EXPERTS, dtype=jnp.float32) * top_w[..., None], axis=1)
    gates = gates.astype(t.dtype)
    y = jnp.zeros_like(t)
    for e in range(N_EXPERTS):
        hid = jax.nn.silu(t @ w_gate[e]) * (t @ w_up[e])
        y = y + gates[:, e:e + 1] * (hid @ w_down[e])
    return y.reshape(b, s, d)


def _normal(k, shape, scale):
    return scale * jax.random.normal(k, shape, jnp.float32)


def _gain(k, shape):
    return 1.0 + 0.02 * jax.random.normal(k, shape, jnp.float32)


def setup_inputs(seed: int = 0) -> dict:
    key = jax.random.key(seed)
    ks = jax.random.split(key, 28)
    L = DEPTH
    x = _normal(ks[0], (BATCH, SEQ, D_MODEL), 1.0)
    mem = _normal(ks[1], (BATCH, MEM_TOKENS, D_MODEL), 1.0)
    start = jax.random.randint(ks[2], (BATCH, 1), 0, MAX_POS_OFFSET, dtype=jnp.int32)
    positions = start + jnp.arange(SEQ, dtype=jnp.int32)[None, :]
    mix_norm_w = _gain(ks[3], (L, D_MODEL))
    w_in = _normal(ks[4], (L, D_MODEL, IN_PROJ_DIM), D_MODEL ** -0.5)
    attn_sinks = _normal(ks[5], (L, N_Q_HEADS), 0.5)
    ssd_conv_w = _normal(ks[6], (L, SSD_CONV, SSD_CONV_DIM), SSD_CONV ** -0.5)
    ssd_conv_b = _normal(ks[7], (L, SSD_CONV_DIM), 0.02)
    dt0 = jnp.exp(jax.random.uniform(ks[8], (L, SSD_HEADS), jnp.float32,
                                     minval=math.log(DT_MIN), maxval=math.log(DT_MAX)))
    ssd_dt_bias = dt0 + jnp.log(-jnp.expm1(-dt0))
    ssd_a_log = jnp.log(jax.random.uniform(ks[9], (L, SSD_HEADS), jnp.float32, minval=1.0, maxval=16.0))
    ssd_d = 1.0 + 0.1 * jax.random.normal(ks[10], (L, SSD_HEADS), jnp.float32)
    attn_out_norm_w = _gain(ks[11], (L, ATTN_WIDTH))
    ssd_out_norm_w = _gain(ks[12], (L, SSD_WIDTH))
    w_out = _normal(ks[13], (L, D_MIX, D_MODEL), D_MIX ** -0.5)
    xattn_norm_w = _gain(ks[14], (L, D_MODEL))
    mem_norm_w = _gain(ks[15], (L, D_MODEL))
    xattn_w_q = _normal(ks[16], (L, D_MODEL, XATTN_WIDTH), D_MODEL ** -0.5)
    xattn_w_kv = _normal(ks[17], (L, D_MODEL, 2 * XATTN_WIDTH), D_MODEL ** -0.5)
    xattn_w_o = _normal(ks[18], (L, XATTN_WIDTH, D_MODEL), XATTN_WIDTH ** -0.5)
    ffn_norm_w = _gain(ks[19], (L, D_MODEL))
    router_group_w = _normal(ks[20], (L, D_MODEL, N_GROUPS), D_MODEL ** -0.5)
    router_group_b = _normal(ks[21], (L, N_GROUPS), 0.01)
    router_expert_w = _normal(ks[22], (L, D_MODEL, N_EXPERTS), D_MODEL ** -0.5)
    router_expert_b = _normal(ks[23], (L, N_EXPERTS), 0.01)
    expert_w_gate = _normal(ks[24], (L, N_EXPERTS, D_MODEL, EXPERT_FF), D_MODEL ** -0.5)
    expert_w_up = _normal(ks[25], (L, N_EXPERTS, D_MODEL, EXPERT_FF), D_MODEL ** -0.5)
    expert_w_down = _normal(ks[26], (L, N_EXPERTS, EXPERT_FF, D_MODEL), EXPERT_FF ** -0.5)
    final_norm_w = _gain(ks[27], (D_MODEL,))
    return {
        'x': x, 'mem': mem, 'positions': positions,
        'mix_norm_w': mix_norm_w, 'w_in': w_in, 'attn_sinks': attn_sinks,
        'ssd_conv_w': ssd_conv_w, 'ssd_conv_b': ssd_conv_b, 'ssd_dt_bias': ssd_dt_bias,
        'ssd_a_log': ssd_a_log, 'ssd_d': ssd_d, 'attn_out_norm_w': attn_out_norm_w,
        'ssd_out_norm_w': ssd_out_norm_w, 'w_out': w_out,
        'xattn_norm_w': xattn_norm_w, 'mem_norm_w': mem_norm_w, 'xattn_w_q': xattn_w_q,
        'xattn_w_kv': xattn_w_kv, 'xattn_w_o': xattn_w_o,
        'ffn_norm_w': ffn_norm_w, 'router_group_w': router_group_w, 'router_group_b': router_group_b,
        'router_expert_w': router_expert_w, 'router_expert_b': router_expert_b,
        'expert_w_gate': expert_w_gate, 'expert_w_up': expert_w_up, 'expert_w_down': expert_w_down,
        'final_norm_w': final_norm_w,
    }


def reference(x, mem, positions, mix_norm_w, w_in, attn_sinks, ssd_conv_w, ssd_conv_b,
              ssd_dt_bias, ssd_a_log, ssd_d, attn_out_norm_w, ssd_out_norm_w, w_out,
              xattn_norm_w, mem_norm_w, xattn_w_q, xattn_w_kv, xattn_w_o,
              ffn_norm_w, router_group_w, router_group_b, router_expert_w, router_expert_b,
              expert_w_gate, expert_w_up, expert_w_down, final_norm_w):
    for l in range(DEPTH):
        x = x + hybrid_mixer(rms_norm(x, mix_norm_w[l]), positions, w_in[l], attn_sinks[l],
                             ssd_conv_w[l], ssd_conv_b[l], ssd_dt_bias[l], ssd_a_log[l], ssd_d[l],
                             attn_out_norm_w[l], ssd_out_norm_w[l], w_out[l])
        x = x + memory_cross_attention(rms_norm(x, xattn_norm_w[l]), rms_norm(mem, mem_norm_w[l]),
                                       xattn_w_q[l], xattn_w_kv[l], xattn_w_o[l])
        x = x + hierarchical_moe(rms_norm(x, ffn_norm_w[l]), router_group_w[l], router_group_b[l],
                                 router_expert_w[l], router_expert_b[l],
                                 expert_w_gate[l], expert_w_up[l], expert_w_down[l])
    return rms_norm(x, final_norm_w)
```

```python
import numpy as np
import concourse.bass as bass
import concourse.mybir as mybir
from concourse.bass_utils import run_bass_kernel_spmd

F32 = mybir.dt.float32
BF16 = mybir.dt.bfloat16
I32 = mybir.dt.int32
AF = mybir.ActivationFunctionType
ALU = mybir.AluOpType
PI = float(np.pi)

D = 1024
NCORES = 8
SEQ = 4096
MEMT = 256
C_MIXW, C_XAW, C_MEMW, C_FFNW, C_CONVB, C_CONVW, C_INVF, NCOL = 0, 8, 16, 24, 32, 40, 72, 73
R_SINK, R_DTB, R_ALOG, R_DSK, R_RB, R_ANW, R_SNW, R_FNW, NROW = 0, 8, 16, 24, 32, 68, 580, 1092, 2116


class Sched:
    def __init__(self, nc):
        self.nc = nc
        self.eng = {"pe": nc.tensor, "act": nc.scalar, "dve": nc.vector,
                    "pool": nc.gpsimd, "sp": nc.sync}
        self.sem = {}
        self.cnt = {}
        for e in self.eng:
            self.sem[e] = nc.alloc_semaphore("s_" + e)
            self.cnt[e] = 0
        self.waited = {e: {} for e in self.eng}
        self.last_w = {}
        self.readers = {}
        self.dsem = {}
        self.dcnt = {}
        self.efree = {e: 0.0 for e in self.eng}
        self.kready = {}
        self.kread_t = {}
        self.last_finish = 0.0

    def _model(self, e, reads, writes, dur, dma=False, occ=0.1):
        lat = 0.35
        t = self.efree[e]
        for b in list(reads) + list(writes):
            t = max(t, self.kready.get(b, 0.0) + lat)
        for b in writes:
            t = max(t, self.kread_t.get(b, 0.0) + lat)
        fin = t + dur
        self.efree[e] = t + occ if dma else fin
        for b in writes:
            self.kready[b] = fin
            self.kread_t[b] = 0.0
        for b in reads:
            self.kread_t[b] = max(self.kread_t.get(b, 0.0), fin)
        self.last_finish = fin

    def _need(self, e, ev):
        if ev is None:
            return
        k, v = ev
        if self.waited[e].get(k, 0) >= v:
            return
        self.waited[e][k] = v
        s = self.sem[k] if k in self.sem else self.dsem[k]
        self.eng[e].wait_ge(s, v)

    def _deps(self, e, reads, writes):
        for b in list(reads) + list(writes):
            self._need(e, self.last_w.get(b))
        for b in list(writes) + [r for r in reads if isinstance(r, str) and r.startswith("ps")]:
            for ev in self.readers.get(b, ()):
                if b in writes or ev[0] != e:
                    self._need(e, ev)

    def _record(self, ev, reads, writes):
        for b in writes:
            self.last_w[b] = ev
            self.readers[b] = []
        for b in reads:
            d = dict(self.readers.get(b, []))
            d[ev[0]] = max(d.get(ev[0], 0), ev[1])
            self.readers[b] = list(d.items())

    def op(self, e, fn, reads=(), writes=(), cost=0.5):
        self._model(e, reads, writes, cost)
        self._deps(e, reads, writes)
        ins = fn(self.eng[e])
        self.cnt[e] += 1
        ins.then_inc(self.sem[e], 1)
        self._record((e, self.cnt[e]), reads, writes)

    def dma(self, q, out, in_, key, reads=(), writes=()):
        nb = 1
        for d_ in out.shape:
            nb *= int(d_)
        nb *= 2 if out.dtype == BF16 else 4
        self._model(q, reads, writes, 2.0 + nb / 150e3, dma=True, occ=(nb / 175e3 if q == "pool" else 0.1))
        dk = ("d", key)
        if dk not in self.dsem:
            self.dsem[dk] = self.nc.alloc_semaphore("d_%d" % len(self.dsem))
            self.dcnt[dk] = 0
        self._deps(q, reads, writes)
        ins = self.eng[q].dma_start(out=out, in_=in_)
        self.dcnt[dk] += 16
        ins.then_inc(self.dsem[dk], 16)
        self._record((dk, self.dcnt[dk]), reads, writes)

    def fence(self, keys):
        for e in self.eng:
            for b in keys:
                self._need(e, self.last_w.get(b))
                for ev in self.readers.get(b, ()):
                    self._need(e, ev)

    def finish(self, q="sp"):
        for e in self.eng:
            if self.cnt[e] > 0:
                self._need(q, (e, self.cnt[e]))
        for dk, v in self.dcnt.items():
            if v > 0:
                self._need(q, (dk, v))


class Arena:
    def view(self, off, shape, dtype):
        save = self.off
        self.off = off
        a = self.alloc(shape, dtype)
        self.off = save
        return a

    def __init__(self, nc, nbytes):
        self.t = nc.alloc_sbuf_tensor("arena", [128, nbytes // 4], F32)
        self.cap = nbytes
        self.off = 0

    def alloc(self, shape, dtype):
        esz = 2 if dtype == BF16 else 4
        n = int(np.prod(shape))
        nb = (n * esz + 31) // 32 * 32
        assert self.off + nb <= self.cap, ("SBUF arena overflow", self.off, nb, self.cap)
        a = self.t[:, self.off // 4:(self.off + nb) // 4]
        self.off += nb
        if dtype != F32:
            a = a.bitcast(dtype)
        a = a[:, 0:n]
        if len(shape) == 2:
            a = a.rearrange("p (a b) -> p a b", a=shape[0])
        elif len(shape) == 3:
            a = a.rearrange("p (a b c) -> p a b c", a=shape[0], b=shape[1])
        return a


def build(T, stop=99):
    class _Stop(Exception):
        pass

    def chk(stage):
        if stop <= stage:
            raise _Stop()

    try:
        return _build(T, chk)
    except _Stop:
        pass
    return _NC[0]


_NC = [None]


def _build(T, chk):
    assert T % 512 == 0
    NCH = T // 128
    NST = T // 512
    TP = min(T, 2048)
    NPASS = T // TP
    NPC = TP // 128
    nc = bass.Bass("TRN2", target_bir_lowering=False)

    def din(name, shape, dt=F32):
        return nc.dram_tensor(name, list(shape), dt, kind="ExternalInput").ap()

    x_d = din("x", [T, D])
    mem_d = din("mem", [MEMT, D])
    pos_d = din("pos", [128, T], I32)
    wf_d = din("wf", [D, 1792])
    wt_d = din("wt", [D, 648])
    wo_d = din("wo", [D, D])
    xq_d = din("xq", [D, 512])
    xkv_d = din("xkv", [D, 1024])
    xo_d = din("xo", [512, D])
    wr_d = din("wr", [D, 36])
    wg_d = din("wg", [32, D, 256])
    wu_d = din("wu", [32, D, 256])
    wd_d = din("wd", [32, 256, D])
    cols_d = din("cols", [128, NCOL])
    rows_d = din("rows", [128, NROW])
    ident_d = din("ident", [128, 128])
    tri_d = din("tri", [128, 128])
    rotm_d = din("rotm", [128, 128])
    out_d = nc.dram_tensor("out", [T, D], F32, kind="ExternalOutput").ap()
    x2_d = nc.dram_tensor("x2scr", [T, D], F32).ap()

    _NC[0] = nc
    S = Sched(nc)
    A = Arena(nc, 206 * 1024)
    _chk = chk

    def chk(stage):
        try:
            _chk(stage)
        except Exception:
            S.finish("sp")
            raise
    pst = nc.alloc_psum_tensor("pst", [128, 4096], F32)
    ps_i = [0]

    ps_busy = [False] * 8

    def PS():
        for _ in range(8):
            i = ps_i[0]
            ps_i[0] = (i + 1) % 8
            if not ps_busy[i]:
                return pst[:, i * 512:(i + 1) * 512], "ps%d" % i
        raise RuntimeError("no free PSUM bank")

    def PSa(n=1):
        while True:
            free = [(ps_i[0] + d) % 8 for d in range(8) if not ps_busy[(ps_i[0] + d) % 8]]
            if len(free) >= n:
                take = free[:n]
                for i in take:
                    ps_busy[i] = True
                ps_i[0] = (take[-1] + 1) % 8
                return [(pst[:, i * 512:(i + 1) * 512], "ps%d" % i) for i in take]
            yield

    def PSr(*keys):
        for k in keys:
            ps_busy[int(k[2:])] = False

    def fsz(ap):
        n = 1
        for d in ap.shape[1:]:
            n *= int(d)
        return n

    def ecost(eng, out):
        n = fsz(out)
        if eng == "act":
            return 0.28 + n / 1200.0
        if eng == "pool":
            return 0.15 + n / 450.0
        return 0.07 + n / 960.0

    def act(out, in_, func, R, W, **kw):
        S.op("act", lambda e: e.activation(out=out, in_=in_, func=func, **kw), R, W, cost=ecost("act", out))

    def tt(eng, out, in0, in1, op, R, W):
        S.op(eng, lambda e: e.tensor_tensor(out=out, in0=in0, in1=in1, op=op), R, W, cost=ecost(eng, out))

    def ts(eng, out, in0, s1, s2, op0, op1, R, W):
        if op1 is None:
            S.op(eng, lambda e: e.tensor_scalar(out=out, in0=in0, scalar1=s1, scalar2=None, op0=op0), R, W,
                 cost=ecost(eng, out))
        else:
            S.op(eng, lambda e: e.tensor_scalar(out=out, in0=in0, scalar1=s1, scalar2=s2, op0=op0, op1=op1), R, W,
                 cost=ecost(eng, out))

    def stt(eng, out, in0, sc, in1, op0, op1, R, W):
        S.op(eng, lambda e: e.scalar_tensor_tensor(out=out, in0=in0, scalar=sc, in1=in1, op0=op0, op1=op1), R, W,
             cost=ecost(eng, out))

    def cp(eng, out, in_, R, W):
        if eng == "act":
            S.op(eng, lambda e: e.activation(out=out, in_=in_, func=AF.Copy), R, W, cost=ecost(eng, out))
        else:
            S.op(eng, lambda e: e.tensor_copy(out=out, in_=in_), R, W, cost=ecost(eng, out))

    def mm(lst, R, W):
        def f(e):
            for (o, l, r, st, sp) in lst:
                i = e.matmul(o, lhsT=l, rhs=r, start=st, stop=sp)
            return i
        c = sum(max(0.06, fsz(r) / 2400.0 + 0.005) * (4.0 if l.dtype == F32 else 1.0) for (o, l, r, st, sp) in lst)
        S.op("pe", f, R, W, cost=c)

    def tr(lst, idn, R, W):
        def f(e):
            for (o, i_) in lst:
                i = e.transpose(out=o, in_=i_, identity=idn)
            return i
        S.op("pe", f, R, W, cost=len(lst) * (0.3 if idn.dtype == F32 else 0.1))

    def recip(out, in_, R, W):
        S.op("dve", lambda e: e.reciprocal(out=out, in_=in_), R, W)

    def memset(eng, ap, v, W):
        S.op(eng, lambda e: e.memset(ap, v), (), W)

    cols = A.alloc([NCOL], F32)
    rows = A.alloc([NROW], F32)
    identf = A.alloc([128], F32)
    identb = A.alloc([128], BF16)
    trif = A.alloc([128], F32)
    trib = A.alloc([128], BF16)
    ntrib = A.alloc([128], BF16)
    ones = A.alloc([128], F32)
    esink = A.alloc([8], F32)
    abc = A.alloc([8], F32)
    junk = A.alloc([1024], F32)
    sm = A.alloc([64], F32)
    KmT = A.alloc([4, 256], BF16)
    Vm = A.alloc([2, 4, 129], BF16)

    S.dma("sp", cols, cols_d, "cols", writes=["cols"])
    S.dma("sp", rows, rows_d, "rows", writes=["rows"])
    S.dma("sp", identf, ident_d, "identf", writes=["identf"])
    S.dma("sp", trif, tri_d, "trif", writes=["trif"])
    cp("dve", identb, identf, ["identf"], ["identb"])
    cp("dve", trib, trif, ["trif"], ["trib"])
    ts("dve", ntrib, trif, -1.0, 1.0, ALU.mult, ALU.add, ["trif"], ["ntrib"])
    memset("dve", ones, 1.0, ["ones"])
    act(esink, rows[:, R_SINK:R_SINK + 8], AF.Exp, ["rows"], ["esink"])
    act(abc, rows[:, R_ALOG:R_ALOG + 8], AF.Exp, ["rows"], ["abc"])
    ts("dve", abc, abc, -1.0, None, ALU.mult, None, ["abc"], ["abc"])
    one_c = ones[:, 0:1]
    rotb = A.alloc([128], BF16)
    S.dma("sp", junk[:, 0:128], rotm_d, "junk", writes=["junk"])
    cp("dve", rotb, junk[:, 0:128], ["junk"], ["rotb"])
    mbias = [A.alloc([2, 128], BF16) for _ in range(2)]
    ts("dve", mbias[0], trif.unsqueeze(1).to_broadcast([128, 2, 128]), -30000.0, None, ALU.mult, None,
       ["trif"], ["mbias"])
    ts("dve", mbias[1], trif.unsqueeze(1).to_broadcast([128, 2, 128]), 30000.0, -30000.0, ALU.mult, ALU.add,
       ["trif"], ["mbias"])
    epsc = A.alloc([1], F32)
    memset("dve", epsc, 1e-5, ["epsc"])
    chk(1)

    def rstd_chain(ss, rstd, inv_n, kss, krs):
        act(rstd, ss, AF.Ln, [kss, "epsc"], [krs], scale=inv_n, bias=epsc)
        act(rstd, rstd, AF.Exp, [krs], [krs], scale=-0.5)

    mark0 = A.off

    Wf = A.alloc([8, 1792], BF16)
    Wt = A.alloc([8, 648], BF16)
    Wo = A.alloc([8, 1024], BF16)
    for kc in range(8):
        S.dma("pool", Wf[:, kc, :], wf_d[kc * 128:(kc + 1) * 128, :], "Wf", writes=["Wf"])
    S.dma("pool", Wt, wt_d.rearrange("(c p) n -> p c n", p=128), "Wt", writes=["Wt"])

    xin = [A.alloc([1024], F32) for _ in range(2)]
    xrs = [A.alloc([1024], F32)]
    xn = [A.alloc([1024], BF16) for _ in range(2)]
    xT = [A.alloc([8, 512], BF16) for _ in range(2)]
    posi = A.alloc([512], I32)
    COS = A.alloc([512], F32)
    SIN = A.alloc([512], F32)
    tgk = A.alloc([512], F32)
    tgi = A.alloc([512], I32)
    qTs = [A.alloc([4, 512], BF16) for _ in range(2)]
    kThs = [A.alloc([2, 640], BF16) for _ in range(2)]
    Vhs = [A.alloc([5, 2, 65], BF16) for _ in range(2)]
    rt1 = A.alloc([512], F32)
    rt2 = A.alloc([512], F32)
    qab = A.alloc([512], BF16)
    tga, tgr = rt1, rt2
    xpre = [A.alloc([515], F32) for _ in range(2)]
    halo = A.alloc([8, 3], F32)
    cacc = [A.alloc([512], F32) for _ in range(2)]
    xacts = [A.alloc([8, 512], BF16) for _ in range(2)]
    xsB2 = [A.alloc([768], BF16) for _ in range(2)]
    zss = [[A.alloc([512], BF16) for _ in range(4)] for _ in range(2)]
    dtts = [[A.alloc([8], F32) for _ in range(4)] for _ in range(2)]
    Pb = [A.alloc([512], BF16) for _ in range(2)]
    attn = A.alloc([512], F32)
    mix = [A.alloc([1024], BF16) for _ in range(2)]
    mixT = A.alloc([8, 128], BF16)
    da = A.alloc([8], F32)
    cumtot = A.alloc([16], F32)
    ecum2 = [A.alloc([16], F32) for _ in range(2)]
    dte = A.alloc([8], F32)
    dtd = A.alloc([8], F32)
    dabc = A.alloc([8, 128], F32)
    segc = A.alloc([8, 128], F32)
    cbm = A.alloc([2, 128], F32)
    MT = A.alloc([8, 128], BF16)
    xc = A.alloc([8, 64], BF16)
    xdte2 = [A.alloc([8, 64], BF16) for _ in range(2)]
    off_y1 = A.off
    y1 = A.alloc([8, 64], F32)
    y2 = A.alloc([8, 64], F32)
    Sst = A.alloc([8, 64], F32)
    Sbf = A.alloc([8, 64], BF16)
    den = A.alloc([8], F32)
    smA = A.alloc([32], F32)

    memset("dve", halo, 0.0, ["halo%d" % c for c in range(8)])
    memset("dve", Sst, 0.0, ["Sst"])
    memset("dve", Sbf, 0.0, ["Sbf"])

    Wkv = Wo
    memT = segc.rearrange("p a b -> p (a b)").bitcast(BF16).rearrange("p (a b) -> p a b", a=8)
    memx = A.view(off_y1, [1024], F32)
    memn = MT.rearrange("p a b -> p (a b)")
    KWKV, KMEMX, KMEMN, KMEMT = ["Wo"], ["y1", "y2"], ["MT"], ["segc"]
    S.dma("pool", Wkv, xkv_d.rearrange("(c p) n -> p c n", p=128), "Wkv", writes=KWKV)
    memset("dve", Vm[:, :, :, 128:129], 1.0, ["Vm"])

    def memkv_gen():
        ssc, rsc = sm[:, 10:11], sm[:, 11:12]
        for m in range(2):
            S.dma("sp", memx, mem_d[m * 128:(m + 1) * 128, :], "memx", writes=KMEMX)
            yield
            act(junk, memx, AF.Square, KMEMX, ["junk", "mkss"], accum_out=ssc)
            yield
            act(rsc, ssc, AF.Ln, ["mkss", "epsc"], ["mkrs"], scale=1.0 / D, bias=epsc)
            yield
            act(rsc, rsc, AF.Exp, ["mkrs"], ["mkrs"], scale=-0.5)
            yield
            act(memn, memx, AF.Copy, KMEMX + ["mkrs"], KMEMN, scale=rsc)
            yield
            (pb, kb), = yield from PSa(1)
            pbb = pb.bitcast(BF16)
            tr([(pbb[:, c * 128:(c + 1) * 128], memn[:, c * 128:(c + 1) * 128]) for c in range(8)], identb,
               KMEMN + ["identb"], [kb])
            yield
            tt("dve", memT[:, :, m * 128:(m + 1) * 128], pbb.rearrange("p (c t) -> p c t", c=8),
               cols[:, C_MEMW:C_MEMW + 8].unsqueeze(2).to_broadcast([128, 8, 128]), ALU.mult,
               [kb, "cols"], KMEMT)
            PSr(kb)
            yield
        for h in range(4):
            (pb, kb), = yield from PSa(1)
            mm([(pb[:, 0:256], Wkv[:, kc, h * 128:(h + 1) * 128], memT[:, kc, :], kc == 0, kc == 7) for kc in range(8)],
               KWKV + KMEMT, [kb])
            yield
            act(KmT[:, h, :], pb[:, 0:256], AF.Copy, [kb], ["KmT"])
            PSr(kb)
            yield
        for m in range(2):
            (pb, kb), = yield from PSa(1)
            mm([(pb, memT[:, kc, m * 128:(m + 1) * 128], Wkv[:, kc, 512:1024], kc == 0, kc == 7) for kc in range(8)],
               KWKV + KMEMT, [kb])
            yield
            act(Vm[:, m, :, 0:128], pb.rearrange("p (h d) -> p h d", h=4), AF.Copy, [kb], ["Vm"])
            PSr(kb)
            yield

    chk(2)

    for q_ in range(2):
        memset("dve", Vhs[q_][:, :, :, 64:65], 1.0, ["Vh%d_%d" % (q_, c) for c in range(5)])
        memset("dve", kThs[q_], 0.0, ["kTh%d" % q_])

    def XT(s_):
        return ["xT%d_%d" % (s_ % 2, j) for j in range(4)]

    def run_streams(gens):
        gens = list(gens)
        clk = {id(g_): 0.0 for g_ in gens}
        idle_sweeps = 0
        while gens:
            progressed = False
            for g_ in sorted(gens, key=lambda x: clk[id(x)]):
                before = (tuple(S.cnt.values()), sum(S.dcnt.values()))
                try:
                    next(g_)
                except StopIteration:
                    gens.remove(g_)
                    progressed = True
                    break
                if (tuple(S.cnt.values()), sum(S.dcnt.values())) != before:
                    clk[id(g_)] = S.last_finish
                    progressed = True
                    break
            idle_sweeps = 0 if progressed else idle_sweeps + 1
            assert idle_sweeps < 10000, "stream scheduler stuck"
        assert not any(ps_busy), "PSUM bank leaked by a stream"

    def rstd_g(ss, rstd, inv_n, kss, krs):
        act(rstd, ss, AF.Ln, [kss, "epsc"], [krs], scale=inv_n, bias=epsc)
        yield
        act(rstd, rstd, AF.Exp, [krs], [krs], scale=-0.5)
        yield

    def a1_gen(s_, j0):
        xt_ = xT[s_ % 2]
        for j in (j0, j0 + 2):
            n = s_ * 4 + j
            kx = "xin%d" % j0
            kss, krs, kxn = "a1ss%d" % j0, "a1rs%d" % j0, "xn%d" % j0
            ssc = smA[:, j0 * 2:j0 * 2 + 1]
            rsc = smA[:, j0 * 2 + 1:j0 * 2 + 2]
            S.dma("sp", xin[j0], x_d[n * 128:(n + 1) * 128, :], kx, writes=[kx])
            yield
            act(junk, xin[j0], AF.Square, [kx], ["junk", kss], accum_out=ssc)
            yield
            yield from rstd_g(ssc, rsc, 1.0 / D, kss, krs)
            act(xn[j0], xin[j0], AF.Copy, [kx, krs], [kxn], scale=rsc)
            yield
            (pb, kb), = yield from PSa(1)
            pbb = pb.bitcast(BF16)
            tr([(pbb[:, c * 128:(c + 1) * 128], xn[j0][:, c * 128:(c + 1) * 128]) for c in range(8)], identb,
               [kxn, "identb"], [kb])
            yield
            tt("dve", xt_[:, :, j * 128:(j + 1) * 128], pbb.rearrange("p (c t) -> p c t", c=8),
               cols[:, C_MIXW:C_MIXW + 8].unsqueeze(2).to_broadcast([128, 8, 128]), ALU.mult,
               [kb, "cols"], [XT(s_)[j]])
            PSr(kb)
            yield

    FL = {}

    def wait(*ks):
        while not all(FL.get(k) for k in ks):
            yield

    def trig_gen(s_):
        yield from wait(("rope", s_ - 1))
        S.dma("sp", posi, pos_d[:, s_ * 512:(s_ + 1) * 512], "posi", writes=["posi"])
        cp("dve", tgk, posi, ["posi"], ["tgk"])
        yield
        ts("dve", tgk, tgk, cols[:, C_INVF:C_INVF + 1], None, ALU.mult, None, ["tgk", "cols"], ["tgk"])
        yield
        for (tab, ktab, shift) in ((SIN, "SIN", 0.0), (COS, "COS", PI / 2)):
            ts("dve", tga, tgk, shift, None, ALU.add, None, ["tgk"], ["rt1"])
            yield
            ts("dve", tgr, tga, 1.0 / (2 * PI), None, ALU.mult, None, ["rt1"], ["rt2"])
            yield
            cp("dve", tgi, tgr, ["rt2"], ["tgi"])
            yield
            cp("dve", tgr, tgi, ["tgi"], ["rt2"])
            yield
            stt("dve", tga, tgr, -2 * PI, tga, ALU.mult, ALU.add, ["rt2", "rt1"], ["rt1"])
            yield
            ts("dve", tga, tga, -PI, PI, ALU.max, ALU.min, ["rt1"], ["rt1"])
            yield
            act(tab, tga, AF.Sin, ["rt1"], [ktab])
            yield
        FL[("trig", s_)] = True

    def proj_fm(s_, c, pb, kb):
        xt_ = xT[s_ % 2]
        mm([(pb, Wf[:, kc, c * 128:(c + 1) * 128], xt_[:, kc, :], kc == 0, kc == 7) for kc in range(8)],
           ["Wf"] + XT(s_), [kb])
        return pb, kb

    def rope_gen(s_):
        q_ = s_ % 2
        qT, kTh = qTs[q_], kThs[q_]
        kqT, kkT = "qT%d" % q_, "kTh%d" % q_
        yield from wait(("trig", s_), ("swa", s_ - 2))
        if s_ > 0:
            cp("pool", kTh[:, :, 0:128], kThs[1 - q_][:, :, 512:640], ["kTh%d" % (1 - q_)], [kkT])
            yield
        items = [(c, qT[:, c, :], kqT) for c in range(4)] + \
                [(4 + g, kTh[:, g, 128:640], kkT) for g in range(2)]
        for (ca, outap, kout) in items:
            (pa, ka), (pb_, kb_) = yield from PSa(2)
            proj_fm(s_, ca, pa, ka)
            yield
            cp("act", qab, pa, [ka], ["qab"])
            yield
            mm([(pb_, rotb, qab, True, True)], ["rotb", "qab"], [kb_])
            yield
            tt("dve", rt1, pa, COS, ALU.mult, [ka, "COS"], ["rt1"])
            yield
            tt("dve", rt2, pb_, SIN, ALU.mult, [kb_, "SIN"], ["rt2"])
            PSr(ka, kb_)
            yield
            tt("pool", outap, rt1, rt2, ALU.add, ["rt1", "rt2"], [kout])
            yield
        FL[("rope", s_)] = True

    def conv_gen(s_, par):
        q_ = s_ % 2
        xact = xacts[q_]
        yield from wait(("ssd", s_ - 2))
        for c in range(par, 8, 2):
            (pa, ka), = yield from PSa(1)
            proj_fm(s_, 6 + c, pa, ka)
            yield
            xp = xpre[par]
            kxp = "xpre%d" % par
            cp("pool", xp[:, 0:3], halo[:, c, :], ["halo%d" % c], [kxp])
            yield
            act(xp[:, 3:515], pa, AF.Copy, [ka], [kxp])
            PSr(ka)
            yield
            cp("pool", halo[:, c, :], xp[:, 512:515], [kxp], ["halo%d" % c])
            yield
            ac = cacc[par]
            kac = "cacc%d" % par
            ts("dve", ac, xp[:, 0:512], cols[:, C_CONVW + c * 4:C_CONVW + c * 4 + 1], None, ALU.mult, None,
               [kxp, "cols"], [kac])
            yield
            for jt in range(1, 4):
                stt("dve", ac, xp[:, jt:jt + 512], cols[:, C_CONVW + c * 4 + jt:C_CONVW + c * 4 + jt + 1], ac,
                    ALU.mult, ALU.add, [kxp, "cols", kac], [kac])
                yield
            act(xact[:, c, :], ac, AF.Silu, [kac, "cols"], ["xact%d_%d" % (q_, c)], bias=cols[:, C_CONVB + c:C_CONVB + c + 1])
            yield
        FL[("conv", s_, par)] = True

    def a3_gen(s_):
        q_ = s_ % 2
        zs, dtt, Vh = zss[q_], dtts[q_], Vhs[q_]
        yield from wait(("swa", s_ - 2), ("ssd", s_ - 2))
        if s_ > 0:
            cp("pool", Vh[:, 0, :, :], Vhs[1 - q_][:, 4, :, :], ["Vh%d_4" % (1 - q_)], ["Vh%d_0" % q_])
            yield
        xt_ = xT[s_ % 2]
        for j in range(4):
            tok = slice(j * 128, (j + 1) * 128)
            (p1, k1), (p2, k2) = yield from PSa(2)
            mm([(p1, xt_[:, kc, tok], Wt[:, kc, 0:512], kc == 0, kc == 7) for kc in range(8)], ["Wt", XT(s_)[j]], [k1])
            yield
            act(zs[j], p1, AF.Silu, [k1], ["zs%d_%d" % (q_, j)])
            PSr(k1)
            yield
            mm([(p2[:, 0:136], xt_[:, kc, tok], Wt[:, kc, 512:648], kc == 0, kc == 7) for kc in range(8)],
               ["Wt", XT(s_)[j]], [k2])
            yield
            cp("dve", Vh[:, 1 + j, :, 0:64], p2[:, 0:128].rearrange("p (g d) -> p g d", g=2), [k2], ["Vh%d_%d" % (q_, 1 + j)])
            yield
            tt("dve", dtt[j], p2[:, 128:136], rows[:, R_DTB:R_DTB + 8], ALU.add, [k2, "rows"], ["dt%d_%d" % (q_, j)])
            PSr(k2)
            yield
        for j in range(4):
            act(dtt[j], dtt[j], AF.Exp, ["dt%d_%d" % (q_, j)], ["dt%d_%d" % (q_, j)])
            yield
            act(dtt[j], dtt[j], AF.Ln, ["dt%d_%d" % (q_, j), "ones"], ["dt%d_%d" % (q_, j)], bias=one_c)
            yield
        FL[("a3", s_)] = True

    def swa_gen(s_):
        q_ = s_ % 2
        qT, kTh, Vh = qTs[q_], kThs[q_], Vhs[q_]
        kqT, kkT = "qT%d" % q_, "kTh%d" % q_
        yield from wait(("rope", s_), ("a3", s_))
        ssc, rsc = smA[:, 8:9], smA[:, 9:10]
        for j in range(4):
            n = s_ * 4 + j
            tok = slice(j * 128, (j + 1) * 128)
            blocks = [0, 1] if n > 0 else [1]
            for g in range(2):
                pS = {}
                banks_ = yield from PSa(2 * len(blocks))
                for bi_, b in enumerate(blocks):
                    pS[b] = (banks_[2 * bi_], banks_[2 * bi_ + 1])
                    kt = slice((b + j) * 128, (b + j + 1) * 128)
                    for hf in range(2):
                        pr = slice(hf * 64, (hf + 1) * 64)
                        mm([(pS[b][hf][0][:, 0:256], identb, mbias[b].rearrange("p i q -> p (i q)"), True, False)] +
                           [(pS[b][hf][0][:, ii * 128:(ii + 1) * 128], kTh[pr, g, kt], qT[pr, 2 * g + ii, tok], False, ii == 1)
                            for ii in range(2)], [kkT, kqT, "identb", "mbias"], [pS[b][hf][1]])
                        yield
                for b in blocks:
                    kp = "P%d" % b
                    for hf in range(2):
                        act(Pb[b][:, hf * 256:(hf + 1) * 256], pS[b][hf][0][:, 0:256], AF.Exp, [pS[b][hf][1]], [kp],
                            scale=0.125)
                        PSr(pS[b][hf][1])
                        yield
                (pO, kO), = yield from PSa(1)
                lst = []
                for i in range(4):
                    for b in blocks:
                        pi = (i % 2) * 2 + i // 2
                        lst.append((pO[:, i * 65:(i + 1) * 65], Pb[b][:, pi * 128:(pi + 1) * 128], Vh[:, j + b, g, :],
                                    b == blocks[0], b == blocks[-1]))
                mm(lst, ["P0", "P1", "Vh%d_%d" % (q_, j), "Vh%d_%d" % (q_, j + 1)], [kO])
                yield
                pOv = pO[:, 0:260].rearrange("p (i d) -> p i d", i=4)
                tt("dve", den[:, 0:4], pOv[:, :, 64], esink[:, g * 4:(g + 1) * 4], ALU.add, [kO, "esink"], ["den"])
                yield
                recip(den[:, 4:8], den[:, 0:4], ["den"], ["den"])
                yield
                tt("dve", attn[:, g * 256:(g + 1) * 256].rearrange("p (i d) -> p i d", i=4), pOv[:, :, 0:64],
                   den[:, 4:8].unsqueeze(2).to_broadcast([128, 4, 64]), ALU.mult, [kO, "den"], ["attn"])
                PSr(kO)
                yield
            act(junk[:, 0:512], attn, AF.Square, ["attn"], ["junk", "swss"], accum_out=ssc)
            yield
            yield from rstd_g(ssc, rsc, 1.0 / 512, "swss", "swrs")
            yield from wait(("op", n - 2))
            stt("dve", mix[j % 2][:, 0:512], attn, rsc, rows[:, R_ANW:R_ANW + 512], ALU.mult, ALU.mult,
                ["attn", "swrs", "rows"], ["mixA%d" % (j % 2)])
            yield
            FL[("swa_c", n)] = True
        FL[("swa", s_)] = True


    PYD = {}

    def ssdF_gen(s_):
        p_ = s_ % 2
        xact, dtt = xacts[p_], dtts[p_]
        XACT = ["xact%d_%d" % (p_, c) for c in range(8)]
        yield from wait(("conv", s_, 0), ("conv", s_, 1), ("a3", s_))
        for j in range(4):
            n = s_ * 4 + j
            q_ = n % 2
            yield from wait(("ssdT", n - 2))
            tok = slice(j * 128, (j + 1) * 128)
            xsB_, kxs = xsB2[q_], "xsB%d" % q_
            xdte_, kxd = xdte2[q_], "xdte%d" % q_
            ecum_, kec = ecum2[q_], "ecum%d" % q_
            kd = "dt%d_%d" % (p_, j)
            tt("dve", da, dtt[j], abc, ALU.mult, [kd, "abc"], ["da"])
            yield
            (pc, kc_), = yield from PSa(1)
            mm([(pc[:, 0:8], trif, da, True, True), (pc[:, 8:16], ones, da, True, True)], ["trif", "ones", "da"], [kc_])
            yield
            cp("dve", cumtot, pc[:, 0:16], [kc_], ["cumtot"])
            PSr(kc_)
            yield
            pR = yield from PSa(2)
            for hb in range(2):
                mm([(pR[hb][0][:, r * 128:(r + 1) * 128], da[:, hb * 4 + r:hb * 4 + r + 1].to_broadcast([128, 128]), trif,
                     True, True) for r in range(4)], ["da", "trif"], [pR[hb][1]])
                yield
            (pb, kb), = yield from PSa(1)
            pbb = pb.bitcast(BF16)
            tr([(pbb[:, c * 128:(c + 1) * 128], xact[:, c, tok]) for c in range(6)], identb, XACT + ["identb"], [kb])
            yield
            act(ecum_, cumtot, AF.Exp, ["cumtot"], [kec])
            yield
            tt("dve", dte, cumtot[:, 8:16], cumtot[:, 0:8], ALU.subtract, ["cumtot"], ["dte"])
            yield
            act(dte, dte, AF.Exp, ["dte"], ["dte"])
            yield
            cp("act", xsB_, pbb[:, 0:768], [kb], [kxs])
            PSr(kb)
            yield
            xs3 = xsB_[:, 0:512].rearrange("p (h d) -> p h d", h=8)
            tt("dve", dtd, dtt[j], dte, ALU.mult, [kd, "dte"], ["dtd"])
            yield
            for h in range(8):
                ts("dve", segc[:, h, :], pR[h // 4][0][:, (h % 4) * 128:(h % 4 + 1) * 128], cumtot[:, h:h + 1], 0.0,
                   ALU.subtract, ALU.min, [pR[h // 4][1], "cumtot"], ["segc"])
                yield
            PSr(pR[0][1], pR[1][1])
            act(segc, segc, AF.Exp, ["segc"], ["segc"])
            yield
            (pcb, kcb), = yield from PSa(1)
            mm([(pcb[:, g * 128:(g + 1) * 128], xact[:, 4 + g, tok], xact[:, 6 + g, tok], True, True) for g in range(2)],
               XACT, [kcb])
            yield
            tt("dve", cbm, pcb[:, 0:256].rearrange("p (g l) -> p g l", g=2),
               trif.unsqueeze(1).to_broadcast([128, 2, 128]), ALU.mult, [kcb, "trif"], ["cbm"])
            PSr(kcb)
            yield
            tt("dve", xc, xs3, dtt[j].unsqueeze(2).to_broadcast([128, 8, 64]), ALU.mult, [kxs, kd], ["xc"])
            yield
            tt("pool", xdte_, xs3, dtd.unsqueeze(2).to_broadcast([128, 8, 64]), ALU.mult, [kxs, "dtd"], [kxd])
            yield
            for g in range(2):
                tt("dve", MT[:, g * 4:(g + 1) * 4, :], segc[:, g * 4:(g + 1) * 4, :],
                   cbm[:, g, :].unsqueeze(1).to_broadcast([128, 4, 128]), ALU.mult, ["segc", "cbm"], ["MT"])
                yield
            (pyd, kyd), = yield from PSa(1)
            mm([(pyd[:, h * 64:(h + 1) * 64], MT[:, h, :], xc[:, h, :], True, True) for h in range(8)],
               ["MT", "xc"], [kyd])
            PYD[n] = (pyd, kyd)
            FL[("ssdF", n)] = True
            yield

    def ssdT_gen(s_):
        p_ = s_ % 2
        xact, zs = xacts[p_], zss[p_]
        XACT = ["xact%d_%d" % (p_, c) for c in range(8)]
        for j in range(4):
            n = s_ * 4 + j
            q_ = n % 2
            yield from wait(("ssdF", n))
            tok = slice(j * 128, (j + 1) * 128)
            xsB_, kxs = xsB2[q_], "xsB%d" % q_
            xdte_, kxd = xdte2[q_], "xdte%d" % q_
            ecum_, kec = ecum2[q_], "ecum%d" % q_
            xs3 = xsB_[:, 0:512].rearrange("p (h d) -> p h d", h=8)
            pyd, kyd = PYD.pop(n)
            (pyo, kyo), (pst_, kst) = yield from PSa(2)
            mm([(pyo[:, g * 256:(g + 1) * 256], xact[:, 6 + g, tok],
                 Sbf[:, g * 4:(g + 1) * 4, :].rearrange("p h d -> p (h d)"), True, True) for g in range(2)],
               XACT + ["Sbf"], [kyo])
            yield
            mm([(pst_[:, g * 256:(g + 1) * 256], xsB_[:, 512 + g * 128:512 + (g + 1) * 128],
                 xdte_[:, g * 4:(g + 1) * 4, :].rearrange("p h d -> p (h d)"), True, True) for g in range(2)],
               [kxs, kxd], [kst])
            yield
            tt("pool", y2, xs3, rows[:, R_DSK:R_DSK + 8].unsqueeze(2).to_broadcast([128, 8, 64]), ALU.mult,
               [kxs, "rows"], ["y2"])
            yield
            tt("dve", y1, pyo.rearrange("p (h d) -> p h d", h=8), ecum_[:, 0:8].unsqueeze(2).to_broadcast([128, 8, 64]),
               ALU.mult, [kyo, kec], ["y1"])
            yield
            tt("dve", y1, y1, pyd.rearrange("p (h d) -> p h d", h=8), ALU.add, ["y1", kyd], ["y1"])
            PSr(kyo, kyd)
            yield
            tt("dve", Sst, Sst, ecum_[:, 8:16].unsqueeze(2).to_broadcast([128, 8, 64]), ALU.mult, ["Sst", kec], ["Sst"])
            yield
            tt("dve", Sst, Sst, pst_.rearrange("p (h d) -> p h d", h=8), ALU.add, ["Sst", kst], ["Sst"])
            PSr(kst)
            yield
            cp("act", Sbf, Sst, ["Sst"], ["Sbf"])
            yield
            tt("dve", y1, y1, y2, ALU.add, ["y1", "y2"], ["y1"])
            yield
            y1f = y1.rearrange("p h d -> p (h d)")
            tt("dve", y1f, y1f, zs[j], ALU.mult, ["y1", "zs%d_%d" % (p_, j)], ["y1"])
            yield
            for g in range(2):
                act(junk[:, 0:256], y1f[:, g * 256:(g + 1) * 256], AF.Square, ["y1"], ["junk", "sdss"],
                    accum_out=smA[:, 12 + g:13 + g])
                yield
            yield from rstd_g(smA[:, 12:14], smA[:, 14:16], 1.0 / 256, "sdss", "sdrs")
            yield from wait(("op", n - 2))
            for g in range(2):
                stt("dve", mix[j % 2][:, 512 + g * 256:512 + (g + 1) * 256], y1f[:, g * 256:(g + 1) * 256],
                    smA[:, 14 + g:15 + g], rows[:, R_SNW + g * 256:R_SNW + (g + 1) * 256], ALU.mult, ALU.mult,
                    ["y1", "sdrs", "rows"], ["mixB%d" % (j % 2)])
                yield
            FL[("ssd_c", n)] = True
            FL[("ssdT", n)] = True
        FL[("ssd", s_)] = True

    def op_gen(s_):
        for j in range(4):
            n = s_ * 4 + j
            kx = "xrs0"
            yield from wait(("swa_c", n), ("ssd_c", n))
            S.dma("sp", xrs[0], x_d[n * 128:(n + 1) * 128, :], kx, writes=[kx])
            (pb, kb), = yield from PSa(1)
            pbb = pb.bitcast(BF16)
            mx_ = mix[j % 2]
            tr([(pbb[:, c * 128:(c + 1) * 128], mx_[:, c * 128:(c + 1) * 128]) for c in range(8)], identb,
               ["mixA%d" % (j % 2), "mixB%d" % (j % 2), "identb"], [kb])
            FL[("op", n)] = True
            yield
            cp("act", mixT.rearrange("p c t -> p (c t)"), pbb, [kb], ["mixT"])
            PSr(kb)
            yield
            for hf in range(2):
                (po, ko), = yield from PSa(1)
                mm([(po, mixT[:, kc, :], Wo[:, kc, hf * 512:(hf + 1) * 512], kc == 0, kc == 7) for kc in range(8)],
                   ["mixT", "Wo"], [ko])
                yield
                tt("dve", xrs[0][:, hf * 512:(hf + 1) * 512], po, xrs[0][:, hf * 512:(hf + 1) * 512], ALU.add,
                   [ko, kx], [kx])
                PSr(ko)
                yield
            S.dma("sp", x2_d[n * 128:(n + 1) * 128, :], xrs[0], kx, reads=[kx], writes=["x2d%d" % n])
            yield

    for k_ in ("swa", "ssd", "op", "ssdT", "rope"):
        FL[(k_, -1)] = True
        FL[(k_, -2)] = True
    run_streams([a1_gen(0, 0), a1_gen(0, 1), trig_gen(0)])
    g0 = [rope_gen(0), conv_gen(0, 0), conv_gen(0, 1), a3_gen(0), memkv_gen()]
    if NST > 1:
        g0 += [a1_gen(1, 0), a1_gen(1, 1), trig_gen(1)]
    run_streams(g0)
    S.dma("pool", Wo, wo_d.rearrange("(c p) n -> p c n", p=128), "Wo", writes=["Wo"])
    for st in range(NST):
        gens = [swa_gen(st), ssdF_gen(st), ssdT_gen(st), op_gen(st)]
        if st + 1 < NST:
            gens += [rope_gen(st + 1), conv_gen(st + 1, 0), conv_gen(st + 1, 1), a3_gen(st + 1)]
        if st + 2 < NST:
            gens += [a1_gen(st + 2, 0), a1_gen(st + 2, 1), trig_gen(st + 2)]
        run_streams(gens)
        chk(8)

    chk(9)
    A_keys = list(S.last_w.keys())
    A.off = mark0
    S.fence(A_keys)

    yacc = A.alloc([NPC, 1024], F32)
    x3nT = A.alloc([8, TP], BF16)
    gates = A.alloc([NPC, 32], F32)
    rstdm = A.alloc([NPC], F32)
    outb = [A.alloc([1024], F32) for _ in range(2)]
    smB = A.alloc([32], F32)
    Wxq = A.alloc([8, 512], BF16)
    Wxo = A.alloc([4, 1024], BF16)
    Wg0 = A.alloc([8, 256], BF16)
    Wu0 = A.alloc([8, 256], BF16)
    Wd0 = A.alloc([2, 1024], BF16)
    mark1 = A.off
    S.dma("pool", Wxq, xq_d.rearrange("(c p) n -> p c n", p=128), "Wxq", writes=["Wxq"])
    S.dma("pool", Wxo, xo_d.rearrange("(c p) n -> p c n", p=128), "Wxo", writes=["Wxo"])

    def load_w0():
        S.dma("pool", Wg0, wg_d[0].rearrange("(c p) f -> p c f", p=128), "We0", writes=["We0"])
        S.dma("pool", Wu0, wu_d[0].rearrange("(c p) f -> p c f", p=128), "We0", writes=["We0"])
        S.dma("pool", Wd0, wd_d[0].rearrange("(c p) n -> p c n", p=128), "We0", writes=["We0"])
    NSTP = NPC // 4

    FN = {}

    def fn_gen(pp):
        ssc, rsc = smB[:, 16:17], smB[:, 17:18]
        for ci in range(NPC):
            n = pp * NPC + ci
            ky = "y%d" % ci
            act(junk, yacc[:, ci, :], AF.Square, [ky], ["junk", "fnss"], accum_out=ssc)
            yield
            yield from rstd_g(ssc, rsc, 1.0 / D, "fnss", "fnrs")
            kob = "outb%d" % (ci % 2)
            stt("dve", outb[ci % 2], yacc[:, ci, :], rsc, rows[:, R_FNW:R_FNW + 1024], ALU.mult, ALU.mult,
                [ky, "fnrs", "rows"], [kob])
            FN[(pp, ci)] = True
            yield
            S.dma("sp", out_d[n * 128:(n + 1) * 128, :], outb[ci % 2], kob, reads=[kob])
            yield

    for p in range(NPASS):
        A.off = mark1
        if p == 0:
            S.fence(list(S.last_w.keys()))
        else:
            S.fence(["We1", "sg0", "sg1", "hid0", "hid1"])
        Wr32 = A.alloc([8, 36], F32)
        x2n = [A.alloc([1024], BF16) for _ in range(2)]
        x3n = [A.alloc([1024], BF16) for _ in range(2)]
        x2nT = A.alloc([8, 512], BF16)
        qxT = A.alloc([4, 512], BF16)
        PxT = [A.alloc([512], BF16) for _ in range(4)]
        ob = [A.alloc([4, 512], BF16) for _ in range(2)]
        oT = [A.alloc([4, 128], BF16) for _ in range(2)]
        x3T32 = [A.alloc([8, 128], F32) for _ in range(2)]
        lgs = [A.alloc([36], F32) for _ in range(2)]
        rsms = [A.alloc([32], F32) for _ in range(2)]
        msks = [A.alloc([32], F32) for _ in range(2)]
        g1s = [A.alloc([32], F32) for _ in range(2)]
        top8s = [A.alloc([8], F32) for _ in range(2)]
        load_w0()
        S.dma("sp", Wr32, wr_d.rearrange("(c p) n -> p c n", p=128), "Wr32", writes=["Wr32"])
        tt("dve", Wr32, Wr32, cols[:, C_FFNW:C_FFNW + 8].unsqueeze(2).to_broadcast([128, 8, 36]), ALU.mult,
           ["Wr32", "cols"], ["Wr32"])
        FB = {}

        def waitb(*ks):
            while not all(FB.get(k) for k in ks):
                yield

        def la_gen(par, p=p):
            ssc, rsc = smB[:, par * 2:par * 2 + 1], smB[:, par * 2 + 1:par * 2 + 2]
            kss, krs, kxn = "lass%d" % par, "lars%d" % par, "x2n%d" % par
            for s_ in range(NSTP):
                for j in (par, par + 2):
                    ci = s_ * 4 + j
                    n = p * NPC + ci
                    ky = "y%d" % ci
                    while p > 0 and not FN.get((p - 1, ci)):
                        yield
                    S.dma("sp", yacc[:, ci, :], x2_d[n * 128:(n + 1) * 128, :], ky, reads=["x2d%d" % n], writes=[ky])
                    yield
                    act(junk, yacc[:, ci, :], AF.Square, [ky], ["junk", kss], accum_out=ssc)
                    yield
                    yield from rstd_g(ssc, rsc, 1.0 / D, kss, krs)
                    act(x2n[par], yacc[:, ci, :], AF.Copy, [ky, krs], [kxn], scale=rsc)
                    yield
                    yield from waitb(("qx", s_ - 1))
                    (pb, kb), = yield from PSa(1)
                    pbb = pb.bitcast(BF16)
                    tr([(pbb[:, c * 128:(c + 1) * 128], x2n[par][:, c * 128:(c + 1) * 128]) for c in range(8)], identb,
                       [kxn, "identb"], [kb])
                    yield
                    tt("dve", x2nT[:, :, j * 128:(j + 1) * 128], pbb.rearrange("p (c t) -> p c t", c=8),
                       cols[:, C_XAW:C_XAW + 8].unsqueeze(2).to_broadcast([128, 8, 128]), ALU.mult,
                       [kb, "cols"], ["x2nT%d" % j])
                    PSr(kb)
                    yield
                FB[("la", s_, par)] = True

        X2 = ["x2nT%d" % j for j in range(4)]

        def xa_gen():
            for s_ in range(NSTP):
                yield from waitb(("la", s_, 0), ("la", s_, 1))
                for h in range(4):
                    (pb, kb), = yield from PSa(1)
                    mm([(pb, Wxq[:, kc, h * 128:(h + 1) * 128], x2nT[:, kc, :], kc == 0, kc == 7) for kc in range(8)],
                       ["Wxq"] + X2, [kb])
                    yield
                    act(qxT[:, h, :], pb, AF.Copy, [kb], ["qxT"])
                    PSr(kb)
                    yield
                FB[("qx", s_)] = True
                yield from waitb(("xo", s_ - 2, 0), ("xo", s_ - 2, 1))
                obs = ob[s_ % 2]
                for h in range(4):
                    for m in range(2):
                        (pb, kb), = yield from PSa(1)
                        mm([(pb, KmT[:, h, m * 128:(m + 1) * 128], qxT[:, h, :], True, True)], ["KmT", "qxT"], [kb])
                        yield
                        act(PxT[(h % 2) * 2 + m], pb, AF.Exp, [kb], ["PxT%d" % ((h % 2) * 2 + m)], scale=float(128 ** -0.5))
                        PSr(kb)
                        yield
                    for j in range(4):
                        (pO, kO), = yield from PSa(1)
                        mm([(pO[:, 0:129], PxT[(h % 2) * 2 + m][:, j * 128:(j + 1) * 128], Vm[:, m, h, :], m == 0, m == 1)
                            for m in range(2)], ["PxT%d" % ((h % 2) * 2), "PxT%d" % ((h % 2) * 2 + 1), "Vm"], [kO])
                        yield
                        recip(smB[:, 8:9], pO[:, 128:129], [kO], ["rd"])
                        yield
                        ts("dve", obs[:, j, h * 128:(h + 1) * 128], pO[:, 0:128], smB[:, 8:9], None, ALU.mult, None,
                           [kO, "rd"], ["ob%d_%d" % (s_ % 2, j)])
                        PSr(kO)
                        yield
                FB[("xa", s_)] = True

        def xo_gen(par):
            ssc = smB[:, 12 + par:13 + par]
            kss = "xoss%d" % par
            lg, rsm, msk, g1, top8 = lgs[par], rsms[par], msks[par], g1s[par], top8s[par]
            klg, krsm, kmsk, kg1, kt8 = "lg%d" % par, "rsm%d" % par, "msk%d" % par, "g1%d" % par, "t8%d" % par
            for s_ in range(NSTP):
                yield from waitb(("xa", s_))
                obs = ob[s_ % 2]
                for j in (par, par + 2):
                    ci = s_ * 4 + j
                    ky = "y%d" % ci
                    kob = "ob%d_%d" % (s_ % 2, j)
                    koT, kx3, k32 = "oT%d" % par, "x3n%d" % par, "x3T%d" % par
                    (pb, kb), = yield from PSa(1)
                    pbb = pb.bitcast(BF16)
                    tr([(pbb[:, c * 128:(c + 1) * 128], obs[:, j, c * 128:(c + 1) * 128]) for c in range(4)], identb,
                       [kob, "identb"], [kb])
                    yield
                    cp("act", oT[par].rearrange("p c t -> p (c t)"), pbb[:, 0:512], [kb], [koT])
                    PSr(kb)
                    yield
                    for hf in range(2):
                        (po, ko), = yield from PSa(1)
                        mm([(po, oT[par][:, kc, :], Wxo[:, kc, hf * 512:(hf + 1) * 512], kc == 0, kc == 3)
                            for kc in range(4)], [koT, "Wxo"], [ko])
                        yield
                        tt("dve", yacc[:, ci, hf * 512:(hf + 1) * 512], po, yacc[:, ci, hf * 512:(hf + 1) * 512], ALU.add,
                           [ko, ky], [ky])
                        PSr(ko)
                        yield
                    act(junk, yacc[:, ci, :], AF.Square, [ky], ["junk", kss], accum_out=ssc)
                    yield
                    yield from rstd_g(ssc, rstdm[:, ci:ci + 1], 1.0 / D, kss, "rstdm%d" % ci)
                    krm = "rstdm%d" % ci
                    act(x3n[par], yacc[:, ci, :], AF.Copy, [ky, krm], [kx3], scale=rstdm[:, ci:ci + 1])
                    yield
                    (pb, kb), = yield from PSa(1)
                    pbb = pb.bitcast(BF16)
                    tr([(pbb[:, c * 128:(c + 1) * 128], x3n[par][:, c * 128:(c + 1) * 128]) for c in range(8)], identb,
                       [kx3, "identb"], [kb])
                    yield
                    tt("dve", x3nT[:, :, ci * 128:(ci + 1) * 128], pbb.rearrange("p (c t) -> p c t", c=8),
                       cols[:, C_FFNW:C_FFNW + 8].unsqueeze(2).to_broadcast([128, 8, 128]), ALU.mult,
                       [kb, "cols"], ["x3nT%d" % (ci // 4)])
                    PSr(kb)
                    yield
                    for hb in range(2):
                        (pb, kb), = yield from PSa(1)
                        tr([(pb[:, c * 128:(c + 1) * 128], yacc[:, ci, (hb * 4 + c) * 128:(hb * 4 + c + 1) * 128])
                            for c in range(4)], identf, [ky, "identf"], [kb])
                        yield
                        act(x3T32[par][:, hb * 4:(hb + 1) * 4, :].rearrange("p c t -> p (c t)"), pb, AF.Copy, [kb], [k32])
                        PSr(kb)
                        yield
                    (pl, kl), = yield from PSa(1)
                    mm([(pl[:, 0:36], x3T32[par][:, kc, :], Wr32[:, kc, :], kc == 0, kc == 7) for kc in range(8)],
                       [k32, "Wr32"], [kl])
                    yield
                    stt("dve", lg, pl[:, 0:36], rstdm[:, ci:ci + 1], rows[:, R_RB:R_RB + 36], ALU.mult, ALU.add,
                        [kl, krm, "rows"], [klg])
                    PSr(kl)
                    yield
                    S.op("dve", lambda e: e.tensor_reduce(out=rsm[:, 0:1], in_=lg[:, 0:4], axis=mybir.AxisListType.X,
                                                          op=ALU.max), [klg], [krsm])
                    yield
                    ts("dve", rsm[:, 1:2], rsm[:, 0:1], -1.0, None, ALU.mult, None, [krsm], [krsm])
                    yield
                    act(rsm[:, 4:8], lg[:, 0:4], AF.Exp, [klg, krsm], [krsm], bias=rsm[:, 1:2], accum_out=rsm[:, 2:3])
                    yield
                    recip(rsm[:, 3:4], rsm[:, 2:3], [krsm], [krsm])
                    yield
                    ts("dve", rsm[:, 8:12], lg[:, 0:4], rsm[:, 0:1], 1e30, ALU.is_ge, ALU.mult, [klg, krsm], [krsm])
                    yield
                    ts("dve", rsm[:, 8:12], rsm[:, 8:12], -1e30, None, ALU.add, None, [krsm], [krsm])
                    yield
                    tt("dve", msk.rearrange("p (g e) -> p g e", g=4), lg[:, 4:36].rearrange("p (g e) -> p g e", g=4),
                       rsm[:, 8:12].unsqueeze(2).to_broadcast([128, 4, 8]), ALU.add, [klg, krsm], [kmsk])
                    yield
                    S.op("dve", lambda e: e.max(out=top8, in_=msk), [kmsk], [kt8])
                    yield
                    tt("dve", rsm[:, 12:13], top8[:, 1:2], top8[:, 0:1], ALU.subtract, [kt8], [krsm])
                    yield
                    act(rsm[:, 12:13], rsm[:, 12:13], AF.Exp, [krsm], [krsm])
                    yield
                    ts("dve", rsm[:, 12:13], rsm[:, 12:13], 1.0, None, ALU.add, None, [krsm], [krsm])
                    yield
                    recip(rsm[:, 13:14], rsm[:, 12:13], [krsm], [krsm])
                    yield
                    tt("dve", rsm[:, 14:15], rsm[:, 13:14], rsm[:, 3:4], ALU.mult, [krsm], [krsm])
                    yield
                    tt("dve", rsm[:, 15:16], rsm[:, 3:4], rsm[:, 14:15], ALU.subtract, [krsm], [krsm])
                    yield
                    ts("dve", g1, msk, top8[:, 0:1], rsm[:, 14:15], ALU.is_equal, ALU.mult, [kmsk, kt8, krsm], [kg1])
                    yield
                    ts("dve", msk, msk, top8[:, 1:2], rsm[:, 15:16], ALU.is_equal, ALU.mult, [kmsk, kt8, krsm], [kmsk])
                    yield
                    tt("dve", gates[:, ci, :], g1, msk, ALU.add, [kg1, kmsk], ["gates"])
                    yield
                FB[("xo", s_, par)] = True

        FB[("qx", -1)] = True
        for q_ in (-1, -2):
            FB[("xo", q_, 0)] = True
            FB[("xo", q_, 1)] = True
        run_streams([la_gen(0), la_gen(1), xa_gen(), xo_gen(0), xo_gen(1)] + ([fn_gen(p - 1)] if p > 0 else []))
        chk(11)

        A.off = mark1
        S.fence(list(S.last_w.keys()))
        Wg = [Wg0, A.alloc([8, 256], BF16)]
        Wu = [Wu0, A.alloc([8, 256], BF16)]
        Wd = [Wd0, A.alloc([2, 1024], BF16)]
        sg = [A.alloc([512], F32) for _ in range(2)]
        hid = [A.alloc([2, 512], BF16) for _ in range(2)]
        hot = 0
        X3 = ["x3nT%d" % i for i in range(NPC // 4)]
        steps = [(e_, st) for e_ in range(32) for st in range(NSTP)]
        gu_i = [0]

        def PSg():
            i = gu_i[0] % 2
            gu_i[0] += 1
            return ((pst[:, (2 * i) * 512:(2 * i + 1) * 512], "ps%d" % (2 * i)),
                    (pst[:, (2 * i + 1) * 512:(2 * i + 2) * 512], "ps%d" % (2 * i + 1)))
        dn_i = [0]

        def PSd():
            i = 4 + dn_i[0] % 4
            dn_i[0] += 1
            return pst[:, i * 512:(i + 1) * 512], "ps%d" % i

        def load_w(e_):
            sl = e_ % 2
            kw = "We%d" % sl
            S.dma("pool", Wg[sl], wg_d[e_].rearrange("(c p) f -> p c f", p=128), kw, writes=[kw])
            S.dma("pool", Wu[sl], wu_d[e_].rearrange("(c p) f -> p c f", p=128), kw, writes=[kw])
            S.dma("pool", Wd[sl], wd_d[e_].rearrange("(c p) n -> p c n", p=128), kw, writes=[kw])

        def gu(step, f):
            e_, st = step
            sl = e_ % 2
            kw = "We%d" % sl
            stok = slice(st * 512, (st + 1) * 512)
            (pg_, kg_), (pu_, ku_) = PSg()
            mm([(pg_, Wg[sl][:, kc, f * 128:(f + 1) * 128], x3nT[:, kc, stok], kc == 0, kc == 7)
                for kc in range(8)], [kw, X3[st]], [kg_])
            mm([(pu_, Wu[sl][:, kc, f * 128:(f + 1) * 128], x3nT[:, kc, stok], kc == 0, kc == 7)
                for kc in range(8)], [kw, X3[st]], [ku_])
            return (pg_, kg_, pu_, ku_)

        pend = {0: gu(steps[0], 0)}
        for k, step in enumerate(steps):
            e_, st = step
            sl = e_ % 2
            kw = "We%d" % sl
            pend[1] = gu(step, 1)
            hh = hid[hot]
            kh = "hid%d" % hot
            hot ^= 1
            for f in range(2):
                pg_, kg_, pu_, ku_ = pend[f]
                act(sg[f], pg_, AF.Silu, [kg_], ["sg%d" % f])
                tt("dve", hh[:, f, :], sg[f], pu_, ALU.mult, ["sg%d" % f, ku_], [kh])
            if k + 1 < len(steps):
                if steps[k + 1][0] != e_:
                    load_w(steps[k + 1][0])
                pend[0] = gu(steps[k + 1], 0)
            for j in range(4):
                ci = st * 4 + j
                ky = "y%d" % ci
                for hf in range(2):
                    po, ko = PSd()
                    mm([(po, hh[:, f, j * 128:(j + 1) * 128], Wd[sl][:, f, hf * 512:(hf + 1) * 512], f == 0, f == 1)
                        for f in range(2)], [kh, kw], [ko])
                    stt("dve", yacc[:, ci, hf * 512:(hf + 1) * 512], po, gates[:, ci, e_:e_ + 1],
                        yacc[:, ci, hf * 512:(hf + 1) * 512], ALU.mult, ALU.add, [ko, "gates", ky], [ky])
        chk(12)
    run_streams([fn_gen(NPASS - 1)])
    S.finish("sp")
    return nc


def _prep_shared(inp):
    f32 = np.float32
    w_in = np.asarray(inp["w_in"], f32)[0]
    q = w_in[:, 0:512]
    k = w_in[:, 512:640]
    v = w_in[:, 640:768]
    z = w_in[:, 768:1280]
    xbc = w_in[:, 1280:2304]
    dt = w_in[:, 2304:2312]

    def rot(wc, nh):
        w3 = wc.reshape(D, nh, 64).copy()
        r = w3.copy()
        r[:, :, 0:8] = w3[:, :, 8:16]
        r[:, :, 8:16] = w3[:, :, 0:8]
        return r.reshape(D, nh * 64)

    qr = rot(q, 8)
    kr = rot(k, 2)
    kd = [np.concatenate([k[:, g * 64:(g + 1) * 64]] * 2, axis=1) for g in range(2)]
    krd = [np.concatenate([kr[:, g * 64:(g + 1) * 64]] * 2, axis=1) for g in range(2)]
    wf = np.ascontiguousarray(np.concatenate([q, kd[0], kd[1], xbc], axis=1))
    rotm = np.zeros((128, 128), f32)
    for m_ in range(128):
        d_ = m_ % 64
        k_ = m_ + 8 if d_ < 8 else (m_ - 8 if d_ < 16 else m_)
        rotm[k_, m_] = 1.0
    wt = np.ascontiguousarray(np.concatenate([z, v, dt], axis=1))
    assert wf.shape == (D, 1792) and wt.shape == (D, 648)

    def colv(vv):
        return np.asarray(vv, f32).reshape(8, 128).T

    cols = np.zeros((128, NCOL), f32)
    cols[:, C_MIXW:C_MIXW + 8] = colv(inp["mix_norm_w"][0])
    cols[:, C_XAW:C_XAW + 8] = colv(inp["xattn_norm_w"][0])
    cols[:, C_MEMW:C_MEMW + 8] = colv(inp["mem_norm_w"][0])
    cols[:, C_FFNW:C_FFNW + 8] = colv(inp["ffn_norm_w"][0])
    cols[:, C_CONVB:C_CONVB + 8] = colv(inp["ssd_conv_b"][0])
    cw = np.asarray(inp["ssd_conv_w"], f32)[0]
    for c in range(8):
        for j in range(4):
            cols[:, C_CONVW + c * 4 + j] = cw[j, c * 128:(c + 1) * 128]
    inv = (np.float32(500000.0) ** (-np.arange(0, 16, 2, dtype=np.float32) / np.float32(16))).astype(f32)
    invf = np.zeros(128, f32)
    for p_ in range(128):
        d_ = p_ % 64
        if d_ < 8:
            invf[p_] = -inv[d_]
        elif d_ < 16:
            invf[p_] = inv[d_ - 8]
    cols[:, C_INVF] = invf

    rows1 = np.zeros(NROW, f32)
    rows1[R_SINK:R_SINK + 8] = inp["attn_sinks"][0]
    rows1[R_DTB:R_DTB + 8] = inp["ssd_dt_bias"][0]
    rows1[R_ALOG:R_ALOG + 8] = inp["ssd_a_log"][0]
    rows1[R_DSK:R_DSK + 8] = inp["ssd_d"][0]
    rows1[R_RB:R_RB + 4] = inp["router_group_b"][0]
    rows1[R_RB + 4:R_RB + 36] = inp["router_expert_b"][0]
    rows1[R_ANW:R_ANW + 512] = inp["attn_out_norm_w"][0]
    rows1[R_SNW:R_SNW + 512] = inp["ssd_out_norm_w"][0]
    rows1[R_FNW:R_FNW + 1024] = inp["final_norm_w"]
    rows = np.ascontiguousarray(np.broadcast_to(rows1, (128, NROW)))
    wr = np.ascontiguousarray(np.concatenate([np.asarray(inp["router_group_w"], f32)[0],
                                              np.asarray(inp["router_expert_w"], f32)[0]], axis=1))
    return {
        "wf": wf, "wt": wt, "wo": np.ascontiguousarray(np.asarray(inp["w_out"], f32)[0]),
        "xq": np.ascontiguousarray(np.asarray(inp["xattn_w_q"], f32)[0]),
        "xkv": np.ascontiguousarray(np.asarray(inp["xattn_w_kv"], f32)[0]),
        "xo": np.ascontiguousarray(np.asarray(inp["xattn_w_o"], f32)[0]),
        "wr": wr,
        "wg": np.ascontiguousarray(np.asarray(inp["expert_w_gate"], f32)[0]),
        "wu": np.ascontiguousarray(np.asarray(inp["expert_w_up"], f32)[0]),
        "wd": np.ascontiguousarray(np.asarray(inp["expert_w_down"], f32)[0]),
        "cols": cols, "rows": rows,
        "ident": np.eye(128, dtype=f32), "tri": np.triu(np.ones((128, 128), f32)), "rotm": rotm,
    }


def run(inp, T, stop=99):
    shared = _prep_shared(inp)
    nc = build(T, stop)
    in_maps = []
    for b in range(NCORES):
        m = dict(shared)
        m["x"] = np.ascontiguousarray(np.asarray(inp["x"], np.float32)[b, :T])
        m["mem"] = np.ascontiguousarray(np.asarray(inp["mem"], np.float32)[b])
        m["pos"] = np.ascontiguousarray(np.broadcast_to(np.asarray(inp["positions"], np.int32)[b, :T][None, :], (128, T)))
        in_maps.append(m)
    res = run_bass_kernel_spmd(nc, in_maps, core_ids=list(range(NCORES)))
    return np.stack([np.asarray(r["out"], np.float32) for r in res.results], axis=0)


def kernel(**inputs):
    return run(inputs, SEQ)
```

```python
import numpy as np
import concourse.bass as bass
import concourse.mybir as mybir
from concourse.bass_utils import run_bass_kernel_spmd

F32 = mybir.dt.float32
BF16 = mybir.dt.bfloat16
I32 = mybir.dt.int32
AF = mybir.ActivationFunctionType
ALU = mybir.AluOpType
PI = float(np.pi)

D = 1024
NCORES = 8
SEQ = 4096
MEMT = 256
C_MIXW, C_XAW, C_MEMW, C_FFNW, C_CONVB, C_CONVW, C_INVF, NCOL = 0, 8, 16, 24, 32, 40, 72, 73
R_SINK, R_DTB, R_ALOG, R_DSK, R_RB, R_ANW, R_SNW, R_FNW, NROW = 0, 8, 16, 24, 32, 68, 580, 1092, 2116


class Sched:
    def __init__(self, nc):
        self.nc = nc
        self.eng = {"pe": nc.tensor, "act": nc.scalar, "dve": nc.vector,
                    "pool": nc.gpsimd, "sp": nc.sync}
        self.sem = {}
        self.cnt = {}
        for e in self.eng:
            self.sem[e] = nc.alloc_semaphore("s_" + e)
            self.cnt[e] = 0
        self.waited = {e: {} for e in self.eng}
        self.last_w = {}
        self.readers = {}
        self.dsem = {}
        self.dcnt = {}
        self.defer = None
        self.efree = {e: 0.0 for e in self.eng}
        self.kready = {}
        self.kread_t = {}
        self.last_finish = 0.0

    def _model(self, e, reads, writes, dur, dma=False, occ=0.1):
        lat = 0.35
        t = self.efree[e]
        for b in list(reads) + list(writes):
            t = max(t, self.kready.get(b, 0.0) + lat)
        for b in writes:
            t = max(t, self.kread_t.get(b, 0.0) + lat)
        fin = t + dur
        self.efree[e] = t + occ if dma else fin
        for b in writes:
            self.kready[b] = fin
            self.kread_t[b] = 0.0
        for b in reads:
            self.kread_t[b] = max(self.kread_t.get(b, 0.0), fin)
        self.last_finish = fin

    def _need(self, e, ev):
        if ev is None:
            return
        k, v = ev
        if self.waited[e].get(k, 0) >= v:
            return
        self.waited[e][k] = v
        s = self.sem[k] if k in self.sem else self.dsem[k]
        if self.defer is not None:
            self.defer[k] = (s, v)
        else:
            self.eng[e].wait_ge(s, v)

    def _deps(self, e, reads, writes):
        for b in list(reads) + list(writes):
            self._need(e, self.last_w.get(b))
        for b in list(writes) + [r for r in reads if isinstance(r, str) and r.startswith("ps")]:
            for ev in self.readers.get(b, ()):
                if b in writes or ev[0] != e:
                    self._need(e, ev)

    def _record(self, ev, reads, writes):
        for b in writes:
            self.last_w[b] = ev
            self.readers[b] = []
        for b in reads:
            d = dict(self.readers.get(b, []))
            d[ev[0]] = max(d.get(ev[0], 0), ev[1])
            self.readers[b] = list(d.items())

    def op(self, e, fn, reads=(), writes=(), cost=0.5):
        self._model(e, reads, writes, cost)
        self.defer = {}
        self._deps(e, reads, writes)
        waits = list(self.defer.values())
        self.defer = None
        for (s_, v_) in waits[:-1]:
            self.eng[e].wait_ge(s_, v_)
        ins = fn(self.eng[e])
        first = ins
        if isinstance(ins, tuple):
            first, ins = ins
        if waits:
            first._wait_ge(*waits[-1])
        self.cnt[e] += 1
        ins.then_inc(self.sem[e], 1)
        self._record((e, self.cnt[e]), reads, writes)

    def dma(self, q, out, in_, key, reads=(), writes=()):
        nb = 1
        for d_ in out.shape:
            nb *= int(d_)
        nb *= 2 if out.dtype == BF16 else 4
        self._model(q, reads, writes, 2.0 + nb / 150e3, dma=True, occ=(nb / 175e3 if q == "pool" else 0.1))
        dk = ("d", key)
        if dk not in self.dsem:
            self.dsem[dk] = self.nc.alloc_semaphore("d_%d" % len(self.dsem))
            self.dcnt[dk] = 0
        self._deps(q, reads, writes)
        ins = self.eng[q].dma_start(out=out, in_=in_)
        self.dcnt[dk] += 16
        ins.then_inc(self.dsem[dk], 16)
        self._record((dk, self.dcnt[dk]), reads, writes)

    def fence(self, keys):
        for e in self.eng:
            for b in keys:
                self._need(e, self.last_w.get(b))
                for ev in self.readers.get(b, ()):
                    self._need(e, ev)

    def finish(self, q="sp"):
        for e in self.eng:
            if self.cnt[e] > 0:
                self._need(q, (e, self.cnt[e]))
        for dk, v in self.dcnt.items():
            if v > 0:
                self._need(q, (dk, v))


class Arena:
    def view(self, off, shape, dtype):
        save = self.off
        self.off = off
        a = self.alloc(shape, dtype)
        self.off = save
        return a

    def __init__(self, nc, nbytes):
        self.t = nc.alloc_sbuf_tensor("arena", [128, nbytes // 4], F32)
        self.cap = nbytes
        self.off = 0

    def alloc(self, shape, dtype):
        esz = 2 if dtype == BF16 else 4
        n = int(np.prod(shape))
        nb = (n * esz + 31) // 32 * 32
        assert self.off + nb <= self.cap, ("SBUF arena overflow", self.off, nb, self.cap)
        a = self.t[:, self.off // 4:(self.off + nb) // 4]
        self.off += nb
        if dtype != F32:
            a = a.bitcast(dtype)
        a = a[:, 0:n]
        if len(shape) == 2:
            a = a.rearrange("p (a b) -> p a b", a=shape[0])
        elif len(shape) == 3:
            a = a.rearrange("p (a b c) -> p a b c", a=shape[0], b=shape[1])
        return a


def build(T, stop=99):
    class _Stop(Exception):
        pass

    def chk(stage):
        if stop <= stage:
            raise _Stop()

    try:
        return _build(T, chk)
    except _Stop:
        pass
    return _NC[0]


_NC = [None]


def _build(T, chk):
    assert T % 512 == 0
    NCH = T // 128
    NST = T // 512
    TP = min(T, 2048)
    NPASS = T // TP
    NPC = TP // 128
    nc = bass.Bass("TRN2", target_bir_lowering=False)

    def din(name, shape, dt=F32):
        return nc.dram_tensor(name, list(shape), dt, kind="ExternalInput").ap()

    x_d = din("x", [T, D])
    mem_d = din("mem", [MEMT, D])
    pos_d = din("pos", [128, T], I32)
    wf_d = din("wf", [D, 1792])
    wt_d = din("wt", [D, 648])
    wo_d = din("wo", [D, D])
    xq_d = din("xq", [D, 512])
    xkv_d = din("xkv", [D, 1024])
    xo_d = din("xo", [512, D])
    wr_d = din("wr", [D, 36])
    wg_d = din("wg", [32, D, 256])
    wu_d = din("wu", [32, D, 256])
    wd_d = din("wd", [32, 256, D])
    cols_d = din("cols", [128, NCOL])
    rows_d = din("rows", [128, NROW])
    ident_d = din("ident", [128, 128])
    tri_d = din("tri", [128, 128])
    rotm_d = din("rotm", [128, 128])
    out_d = nc.dram_tensor("out", [T, D], F32, kind="ExternalOutput").ap()
    x2_d = nc.dram_tensor("x2scr", [T, D], F32).ap()

    _NC[0] = nc
    S = Sched(nc)
    A = Arena(nc, 206 * 1024)
    _chk = chk

    def chk(stage):
        try:
            _chk(stage)
        except Exception:
            S.finish("sp")
            raise
    pst = nc.alloc_psum_tensor("pst", [128, 4096], F32)
    ps_i = [0]

    ps_busy = [False] * 8

    def PS():
        for _ in range(8):
            i = ps_i[0]
            ps_i[0] = (i + 1) % 8
            if not ps_busy[i]:
                return pst[:, i * 512:(i + 1) * 512], "ps%d" % i
        raise RuntimeError("no free PSUM bank")

    def PSa(n=1):
        while True:
            free = [(ps_i[0] + d) % 8 for d in range(8) if not ps_busy[(ps_i[0] + d) % 8]]
            if len(free) >= n:
                take = free[:n]
                for i in take:
                    ps_busy[i] = True
                ps_i[0] = (take[-1] + 1) % 8
                return [(pst[:, i * 512:(i + 1) * 512], "ps%d" % i) for i in take]
            yield

    def PSr(*keys):
        for k in keys:
            ps_busy[int(k[2:])] = False

    def fsz(ap):
        n = 1
        for d in ap.shape[1:]:
            n *= int(d)
        return n

    def ecost(eng, out):
        n = fsz(out)
        if eng == "act":
            return 0.28 + n / 1200.0
        if eng == "pool":
            return 0.15 + n / 450.0
        return 0.07 + n / 960.0

    def act(out, in_, func, R, W, **kw):
        S.op("act", lambda e: e.activation(out=out, in_=in_, func=func, **kw), R, W, cost=ecost("act", out))

    def tt(eng, out, in0, in1, op, R, W):
        S.op(eng, lambda e: e.tensor_tensor(out=out, in0=in0, in1=in1, op=op), R, W, cost=ecost(eng, out))

    def ts(eng, out, in0, s1, s2, op0, op1, R, W):
        if op1 is None:
            S.op(eng, lambda e: e.tensor_scalar(out=out, in0=in0, scalar1=s1, scalar2=None, op0=op0), R, W,
                 cost=ecost(eng, out))
        else:
            S.op(eng, lambda e: e.tensor_scalar(out=out, in0=in0, scalar1=s1, scalar2=s2, op0=op0, op1=op1), R, W,
                 cost=ecost(eng, out))

    def stt(eng, out, in0, sc, in1, op0, op1, R, W):
        S.op(eng, lambda e: e.scalar_tensor_tensor(out=out, in0=in0, scalar=sc, in1=in1, op0=op0, op1=op1), R, W,
             cost=ecost(eng, out))

    def cp(eng, out, in_, R, W):
        if eng == "act":
            S.op(eng, lambda e: e.activation(out=out, in_=in_, func=AF.Copy), R, W, cost=ecost(eng, out))
        else:
            S.op(eng, lambda e: e.tensor_copy(out=out, in_=in_), R, W, cost=ecost(eng, out))

    def mm(lst, R, W):
        def f(e):
            first = None
            for (o, l, r, st, sp) in lst:
                i = e.matmul(o, lhsT=l, rhs=r, start=st, stop=sp)
                first = first or i
            return (first, i)
        c = sum(max(0.06, fsz(r) / 2400.0 + 0.005) * (4.0 if l.dtype == F32 else 1.0) for (o, l, r, st, sp) in lst)
        S.op("pe", f, R, W, cost=c)

    def tr(lst, idn, R, W):
        def f(e):
            first = None
            for (o, i_) in lst:
                i = e.transpose(out=o, in_=i_, identity=idn)
                first = first or i
            return (first, i)
        S.op("pe", f, R, W, cost=len(lst) * (0.3 if idn.dtype == F32 else 0.1))

    def recip(out, in_, R, W):
        S.op("dve", lambda e: e.reciprocal(out=out, in_=in_), R, W)

    def memset(eng, ap, v, W):
        S.op(eng, lambda e: e.memset(ap, v), (), W)

    cols = A.alloc([NCOL], F32)
    rows = A.alloc([NROW], F32)
    identf = A.alloc([128], F32)
    identb = A.alloc([128], BF16)
    trif = A.alloc([128], F32)
    trib = A.alloc([128], BF16)
    ntrib = A.alloc([128], BF16)
    ones = A.alloc([128], F32)
    esink = A.alloc([8], F32)
    abc = A.alloc([8], F32)
    junk = A.alloc([1024], F32)
    sm = A.alloc([64], F32)
    KmT = A.alloc([4, 256], BF16)
    Vm = A.alloc([2, 4, 129], BF16)

    S.dma("sp", cols, cols_d, "cols", writes=["cols"])
    S.dma("sp", rows, rows_d, "rows", writes=["rows"])
    S.dma("sp", identf, ident_d, "identf", writes=["identf"])
    S.dma("sp", trif, tri_d, "trif", writes=["trif"])
    cp("dve", identb, identf, ["identf"], ["identb"])
    cp("dve", trib, trif, ["trif"], ["trib"])
    ts("dve", ntrib, trif, -1.0, 1.0, ALU.mult, ALU.add, ["trif"], ["ntrib"])
    memset("dve", ones, 1.0, ["ones"])
    act(esink, rows[:, R_SINK:R_SINK + 8], AF.Exp, ["rows"], ["esink"])
    act(abc, rows[:, R_ALOG:R_ALOG + 8], AF.Exp, ["rows"], ["abc"])
    ts("dve", abc, abc, -1.0, None, ALU.mult, None, ["abc"], ["abc"])
    one_c = ones[:, 0:1]
    rotb = A.alloc([128], BF16)
    S.dma("sp", junk[:, 0:128], rotm_d, "junk", writes=["junk"])
    cp("dve", rotb, junk[:, 0:128], ["junk"], ["rotb"])
    mbias = [A.alloc([2, 128], BF16) for _ in range(2)]
    ts("dve", mbias[0], trif.unsqueeze(1).to_broadcast([128, 2, 128]), -30000.0, None, ALU.mult, None,
       ["trif"], ["mbias"])
    ts("dve", mbias[1], trif.unsqueeze(1).to_broadcast([128, 2, 128]), 30000.0, -30000.0, ALU.mult, ALU.add,
       ["trif"], ["mbias"])
    epsc = A.alloc([1], F32)
    memset("dve", epsc, 1e-5, ["epsc"])
    chk(1)

    def rstd_chain(ss, rstd, inv_n, kss, krs):
        act(rstd, ss, AF.Ln, [kss, "epsc"], [krs], scale=inv_n, bias=epsc)
        act(rstd, rstd, AF.Exp, [krs], [krs], scale=-0.5)

    mark0 = A.off

    Wf = A.alloc([8, 1792], BF16)
    Wt = A.alloc([8, 648], BF16)
    Wo = A.alloc([8, 1024], BF16)
    for kc in range(8):
        S.dma("pool", Wf[:, kc, :], wf_d[kc * 128:(kc + 1) * 128, :], "Wf", writes=["Wf"])
    S.dma("pool", Wt, wt_d.rearrange("(c p) n -> p c n", p=128), "Wt", writes=["Wt"])

    xin = [A.alloc([1024], F32) for _ in range(2)]
    xrs = [A.alloc([1024], F32)]
    xn = [A.alloc([1024], BF16) for _ in range(2)]
    xT = [A.alloc([8, 512], BF16) for _ in range(2)]
    posi = A.alloc([512], I32)
    COS = A.alloc([512], F32)
    SIN = A.alloc([512], F32)
    tgk = A.alloc([512], F32)
    tgi = A.alloc([512], I32)
    qTs = [A.alloc([4, 512], BF16) for _ in range(2)]
    kThs = [A.alloc([2, 640], BF16) for _ in range(2)]
    Vhs = [A.alloc([5, 2, 65], BF16) for _ in range(2)]
    rt1 = A.alloc([512], F32)
    rt2 = A.alloc([512], F32)
    qab = A.alloc([512], BF16)
    tga, tgr = rt1, rt2
    xpre = [A.alloc([515], F32) for _ in range(2)]
    halo = A.alloc([8, 3], F32)
    cacc = [A.alloc([512], F32) for _ in range(2)]
    xacts = [A.alloc([8, 512], BF16) for _ in range(2)]
    xsB2 = [A.alloc([768], BF16) for _ in range(2)]
    zss = [[A.alloc([512], BF16) for _ in range(4)] for _ in range(2)]
    dtts = [[A.alloc([8], F32) for _ in range(4)] for _ in range(2)]
    Pb = [A.alloc([512], BF16) for _ in range(2)]
    attn = A.alloc([512], F32)
    mix = [A.alloc([1024], BF16) for _ in range(2)]
    mixT = A.alloc([8, 128], BF16)
    da = A.alloc([8], F32)
    cumtot = A.alloc([16], F32)
    ecum2 = [A.alloc([16], F32) for _ in range(2)]
    dte = A.alloc([8], F32)
    dtd = A.alloc([8], F32)
    dabc = A.alloc([8, 128], F32)
    segc = A.alloc([8, 128], F32)
    cbm = A.alloc([2, 128], F32)
    MT = A.alloc([8, 128], BF16)
    xc = A.alloc([8, 64], BF16)
    xdte2 = [A.alloc([8, 64], BF16) for _ in range(2)]
    off_y1 = A.off
    y1 = A.alloc([8, 64], F32)
    y2 = A.alloc([8, 64], F32)
    Sst = A.alloc([8, 64], F32)
    Sbf = A.alloc([8, 64], BF16)
    den = A.alloc([8], F32)
    smA = A.alloc([32], F32)

    memset("dve", halo, 0.0, ["halo%d" % c for c in range(8)])
    memset("dve", Sst, 0.0, ["Sst"])
    memset("dve", Sbf, 0.0, ["Sbf"])

    Wkv = Wo
    memT = segc.rearrange("p a b -> p (a b)").bitcast(BF16).rearrange("p (a b) -> p a b", a=8)
    memx = A.view(off_y1, [1024], F32)
    memn = MT.rearrange("p a b -> p (a b)")
    KWKV, KMEMX, KMEMN, KMEMT = ["Wo"], ["y1", "y2"], ["MT"], ["segc"]
    S.dma("pool", Wkv, xkv_d.rearrange("(c p) n -> p c n", p=128), "Wkv", writes=KWKV)
    memset("dve", Vm[:, :, :, 128:129], 1.0, ["Vm"])

    def memkv_gen():
        ssc, rsc = sm[:, 10:11], sm[:, 11:12]
        for m in range(2):
            S.dma("sp", memx, mem_d[m * 128:(m + 1) * 128, :], "memx", writes=KMEMX)
            yield
            act(junk, memx, AF.Square, KMEMX, ["junk", "mkss"], accum_out=ssc)
            yield
            act(rsc, ssc, AF.Ln, ["mkss", "epsc"], ["mkrs"], scale=1.0 / D, bias=epsc)
            yield
            act(rsc, rsc, AF.Exp, ["mkrs"], ["mkrs"], scale=-0.5)
            yield
            act(memn, memx, AF.Copy, KMEMX + ["mkrs"], KMEMN, scale=rsc)
            yield
            (pb, kb), = yield from PSa(1)
            pbb = pb.bitcast(BF16)
            tr([(pbb[:, c * 128:(c + 1) * 128], memn[:, c * 128:(c + 1) * 128]) for c in range(8)], identb,
               KMEMN + ["identb"], [kb])
            yield
            tt("dve", memT[:, :, m * 128:(m + 1) * 128], pbb.rearrange("p (c t) -> p c t", c=8),
               cols[:, C_MEMW:C_MEMW + 8].unsqueeze(2).to_broadcast([128, 8, 128]), ALU.mult,
               [kb, "cols"], KMEMT)
            PSr(kb)
            yield
        for h in range(4):
            (pb, kb), = yield from PSa(1)
            mm([(pb[:, 0:256], Wkv[:, kc, h * 128:(h + 1) * 128], memT[:, kc, :], kc == 0, kc == 7) for kc in range(8)],
               KWKV + KMEMT, [kb])
            yield
            act(KmT[:, h, :], pb[:, 0:256], AF.Copy, [kb], ["KmT"])
            PSr(kb)
            yield
        for m in range(2):
            (pb, kb), = yield from PSa(1)
            mm([(pb, memT[:, kc, m * 128:(m + 1) * 128], Wkv[:, kc, 512:1024], kc == 0, kc == 7) for kc in range(8)],
               KWKV + KMEMT, [kb])
            yield
            act(Vm[:, m, :, 0:128], pb.rearrange("p (h d) -> p h d", h=4), AF.Copy, [kb], ["Vm"])
            PSr(kb)
            yield

    chk(2)

    for q_ in range(2):
        memset("dve", Vhs[q_][:, :, :, 64:65], 1.0, ["Vh%d_%d" % (q_, c) for c in range(5)])
        memset("dve", kThs[q_], 0.0, ["kTh%d" % q_])

    def XT(s_):
        return ["xT%d_%d" % (s_ % 2, j) for j in range(4)]

    def run_streams(gens):
        gens = list(gens)
        clk = {id(g_): 0.0 for g_ in gens}
        idle_sweeps = 0
        while gens:
            progressed = False
            for g_ in sorted(gens, key=lambda x: clk[id(x)]):
                before = (tuple(S.cnt.values()), sum(S.dcnt.values()))
                try:
                    next(g_)
                except StopIteration:
                    gens.remove(g_)
                    progressed = True
                    break
                if (tuple(S.cnt.values()), sum(S.dcnt.values())) != before:
                    clk[id(g_)] = S.last_finish
                    progressed = True
                    break
            idle_sweeps = 0 if progressed else idle_sweeps + 1
            assert idle_sweeps < 10000, "stream scheduler stuck"
        assert not any(ps_busy), "PSUM bank leaked by a stream"

    def rstd_g(ss, rstd, inv_n, kss, krs):
        act(rstd, ss, AF.Ln, [kss, "epsc"], [krs], scale=inv_n, bias=epsc)
        yield
        act(rstd, rstd, AF.Exp, [krs], [krs], scale=-0.5)
        yield

    def a1_gen(s_, j0):
        xt_ = xT[s_ % 2]
        for j in (j0, j0 + 2):
            n = s_ * 4 + j
            kx = "xin%d" % j0
            kss, krs, kxn = "a1ss%d" % j0, "a1rs%d" % j0, "xn%d" % j0
            ssc = smA[:, j0 * 2:j0 * 2 + 1]
            rsc = smA[:, j0 * 2 + 1:j0 * 2 + 2]
            S.dma("sp", xin[j0], x_d[n * 128:(n + 1) * 128, :], kx, writes=[kx])
            yield
            act(junk, xin[j0], AF.Square, [kx], ["junk", kss], accum_out=ssc)
            yield
            yield from rstd_g(ssc, rsc, 1.0 / D, kss, krs)
            act(xn[j0], xin[j0], AF.Copy, [kx, krs], [kxn], scale=rsc)
            yield
            (pb, kb), = yield from PSa(1)
            pbb = pb.bitcast(BF16)
            tr([(pbb[:, c * 128:(c + 1) * 128], xn[j0][:, c * 128:(c + 1) * 128]) for c in range(8)], identb,
               [kxn, "identb"], [kb])
            yield
            tt("dve", xt_[:, :, j * 128:(j + 1) * 128], pbb.rearrange("p (c t) -> p c t", c=8),
               cols[:, C_MIXW:C_MIXW + 8].unsqueeze(2).to_broadcast([128, 8, 128]), ALU.mult,
               [kb, "cols"], [XT(s_)[j]])
            PSr(kb)
            yield

    FL = {}

    def wait(*ks):
        while not all(FL.get(k) for k in ks):
            yield

    def trig_gen(s_):
        yield from wait(("rope", s_ - 1))
        S.dma("sp", posi, pos_d[:, s_ * 512:(s_ + 1) * 512], "posi", writes=["posi"])
        cp("dve", tgk, posi, ["posi"], ["tgk"])
        yield
        ts("dve", tgk, tgk, cols[:, C_INVF:C_INVF + 1], None, ALU.mult, None, ["tgk", "cols"], ["tgk"])
        yield
        for (tab, ktab, shift) in ((SIN, "SIN", 0.0), (COS, "COS", PI / 2)):
            ts("dve", tga, tgk, shift, None, ALU.add, None, ["tgk"], ["rt1"])
            yield
            ts("dve", tgr, tga, 1.0 / (2 * PI), None, ALU.mult, None, ["rt1"], ["rt2"])
            yield
            cp("dve", tgi, tgr, ["rt2"], ["tgi"])
            yield
            cp("dve", tgr, tgi, ["tgi"], ["rt2"])
            yield
            stt("dve", tga, tgr, -2 * PI, tga, ALU.mult, ALU.add, ["rt2", "rt1"], ["rt1"])
            yield
            ts("dve", tga, tga, -PI, PI, ALU.max, ALU.min, ["rt1"], ["rt1"])
            yield
            act(tab, tga, AF.Sin, ["rt1"], [ktab])
            yield
        FL[("trig", s_)] = True

    def proj_fm(s_, c, pb, kb):
        xt_ = xT[s_ % 2]
        mm([(pb, Wf[:, kc, c * 128:(c + 1) * 128], xt_[:, kc, :], kc == 0, kc == 7) for kc in range(8)],
           ["Wf"] + XT(s_), [kb])
        return pb, kb

    def rope_gen(s_):
        q_ = s_ % 2
        qT, kTh = qTs[q_], kThs[q_]
        kqT, kkT = "qT%d" % q_, "kTh%d" % q_
        yield from wait(("trig", s_), ("swa", s_ - 2))
        if s_ > 0:
            cp("pool", kTh[:, :, 0:128], kThs[1 - q_][:, :, 512:640], ["kTh%d" % (1 - q_)], [kkT])
            yield
        items = [(c, qT[:, c, :], kqT) for c in range(4)] + \
                [(4 + g, kTh[:, g, 128:640], kkT) for g in range(2)]
        for (ca, outap, kout) in items:
            (pa, ka), (pb_, kb_) = yield from PSa(2)
            proj_fm(s_, ca, pa, ka)
            yield
            cp("act", qab, pa, [ka], ["qab"])
            yield
            mm([(pb_, rotb, qab, True, True)], ["rotb", "qab"], [kb_])
            yield
            tt("dve", rt1, pa, COS, ALU.mult, [ka, "COS"], ["rt1"])
            yield
            tt("dve", rt2, pb_, SIN, ALU.mult, [kb_, "SIN"], ["rt2"])
            PSr(ka, kb_)
            yield
            tt("pool", outap, rt1, rt2, ALU.add, ["rt1", "rt2"], [kout])
            yield
        FL[("rope", s_)] = True

    def conv_gen(s_, par):
        q_ = s_ % 2
        xact = xacts[q_]
        yield from wait(("ssd", s_ - 2))
        for c in range(par, 8, 2):
            (pa, ka), = yield from PSa(1)
            proj_fm(s_, 6 + c, pa, ka)
            yield
            xp = xpre[par]
            kxp = "xpre%d" % par
            cp("pool", xp[:, 0:3], halo[:, c, :], ["halo%d" % c], [kxp])
            yield
            act(xp[:, 3:515], pa, AF.Copy, [ka], [kxp])
            PSr(ka)
            yield
            cp("pool", halo[:, c, :], xp[:, 512:515], [kxp], ["halo%d" % c])
            yield
            ac = cacc[par]
            kac = "cacc%d" % par
            ts("dve", ac, xp[:, 0:512], cols[:, C_CONVW + c * 4:C_CONVW + c * 4 + 1], None, ALU.mult, None,
               [kxp, "cols"], [kac])
            yield
            for jt in range(1, 4):
                stt("dve", ac, xp[:, jt:jt + 512], cols[:, C_CONVW + c * 4 + jt:C_CONVW + c * 4 + jt + 1], ac,
                    ALU.mult, ALU.add, [kxp, "cols", kac], [kac])
                yield
            act(xact[:, c, :], ac, AF.Silu, [kac, "cols"], ["xact%d_%d" % (q_, c)], bias=cols[:, C_CONVB + c:C_CONVB + c + 1])
            yield
        FL[("conv", s_, par)] = True

    def a3_gen(s_):
        q_ = s_ % 2
        zs, dtt, Vh = zss[q_], dtts[q_], Vhs[q_]
        yield from wait(("swa", s_ - 2), ("ssd", s_ - 2))
        if s_ > 0:
            cp("pool", Vh[:, 0, :, :], Vhs[1 - q_][:, 4, :, :], ["Vh%d_4" % (1 - q_)], ["Vh%d_0" % q_])
            yield
        xt_ = xT[s_ % 2]
        for j in range(4):
            tok = slice(j * 128, (j + 1) * 128)
            (p1, k1), (p2, k2) = yield from PSa(2)
            mm([(p1, xt_[:, kc, tok], Wt[:, kc, 0:512], kc == 0, kc == 7) for kc in range(8)], ["Wt", XT(s_)[j]], [k1])
            yield
            act(zs[j], p1, AF.Silu, [k1], ["zs%d_%d" % (q_, j)])
            PSr(k1)
            yield
            mm([(p2[:, 0:136], xt_[:, kc, tok], Wt[:, kc, 512:648], kc == 0, kc == 7) for kc in range(8)],
               ["Wt", XT(s_)[j]], [k2])
            yield
            cp("dve", Vh[:, 1 + j, :, 0:64], p2[:, 0:128].rearrange("p (g d) -> p g d", g=2), [k2], ["Vh%d_%d" % (q_, 1 + j)])
            yield
            tt("dve", dtt[j], p2[:, 128:136], rows[:, R_DTB:R_DTB + 8], ALU.add, [k2, "rows"], ["dt%d_%d" % (q_, j)])
            PSr(k2)
            yield
        for j in range(4):
            act(dtt[j], dtt[j], AF.Exp, ["dt%d_%d" % (q_, j)], ["dt%d_%d" % (q_, j)])
            yield
            act(dtt[j], dtt[j], AF.Ln, ["dt%d_%d" % (q_, j), "ones"], ["dt%d_%d" % (q_, j)], bias=one_c)
            yield
        FL[("a3", s_)] = True

    def swa_gen(s_):
        q_ = s_ % 2
        qT, kTh, Vh = qTs[q_], kThs[q_], Vhs[q_]
        kqT, kkT = "qT%d" % q_, "kTh%d" % q_
        yield from wait(("rope", s_), ("a3", s_))
        ssc, rsc = smA[:, 8:9], smA[:, 9:10]
        for j in range(4):
            n = s_ * 4 + j
            tok = slice(j * 128, (j + 1) * 128)
            blocks = [0, 1] if n > 0 else [1]
            for g in range(2):
                pS = {}
                banks_ = yield from PSa(2 * len(blocks))
                for bi_, b in enumerate(blocks):
                    pS[b] = (banks_[2 * bi_], banks_[2 * bi_ + 1])
                    kt = slice((b + j) * 128, (b + j + 1) * 128)
                    for hf in range(2):
                        pr = slice(hf * 64, (hf + 1) * 64)
                        mm([(pS[b][hf][0][:, 0:256], identb, mbias[b].rearrange("p i q -> p (i q)"), True, False)] +
                           [(pS[b][hf][0][:, ii * 128:(ii + 1) * 128], kTh[pr, g, kt], qT[pr, 2 * g + ii, tok], False, ii == 1)
                            for ii in range(2)], [kkT, kqT, "identb", "mbias"], [pS[b][hf][1]])
                        yield
                for b in blocks:
                    kp = "P%d" % b
                    for hf in range(2):
                        act(Pb[b][:, hf * 256:(hf + 1) * 256], pS[b][hf][0][:, 0:256], AF.Exp, [pS[b][hf][1]], [kp],
                            scale=0.125)
                        PSr(pS[b][hf][1])
                        yield
                (pO, kO), = yield from PSa(1)
                lst = []
                for i in range(4):
                    for b in blocks:
                        pi = (i % 2) * 2 + i // 2
                        lst.append((pO[:, i * 65:(i + 1) * 65], Pb[b][:, pi * 128:(pi + 1) * 128], Vh[:, j + b, g, :],
                                    b == blocks[0], b == blocks[-1]))
                mm(lst, ["P0", "P1", "Vh%d_%d" % (q_, j), "Vh%d_%d" % (q_, j + 1)], [kO])
                yield
                pOv = pO[:, 0:260].rearrange("p (i d) -> p i d", i=4)
                tt("dve", den[:, 0:4], pOv[:, :, 64], esink[:, g * 4:(g + 1) * 4], ALU.add, [kO, "esink"], ["den"])
                yield
                recip(den[:, 4:8], den[:, 0:4], ["den"], ["den"])
                yield
                tt("dve", attn[:, g * 256:(g + 1) * 256].rearrange("p (i d) -> p i d", i=4), pOv[:, :, 0:64],
                   den[:, 4:8].unsqueeze(2).to_broadcast([128, 4, 64]), ALU.mult, [kO, "den"], ["attn"])
                PSr(kO)
                yield
            act(junk[:, 0:512], attn, AF.Square, ["attn"], ["junk", "swss"], accum_out=ssc)
            yield
            yield from rstd_g(ssc, rsc, 1.0 / 512, "swss", "swrs")
            yield from wait(("op", n - 2))
            stt("dve", mix[j % 2][:, 0:512], attn, rsc, rows[:, R_ANW:R_ANW + 512], ALU.mult, ALU.mult,
                ["attn", "swrs", "rows"], ["mixA%d" % (j % 2)])
            yield
            FL[("swa_c", n)] = True
        FL[("swa", s_)] = True


    PYD = {}

    def ssdF_gen(s_):
        p_ = s_ % 2
        xact, dtt = xacts[p_], dtts[p_]
        XACT = ["xact%d_%d" % (p_, c) for c in range(8)]
        yield from wait(("conv", s_, 0), ("conv", s_, 1), ("a3", s_))
        for j in range(4):
            n = s_ * 4 + j
            q_ = n % 2
            yield from wait(("ssdT", n - 2))
            tok = slice(j * 128, (j + 1) * 128)
            xsB_, kxs = xsB2[q_], "xsB%d" % q_
            xdte_, kxd = xdte2[q_], "xdte%d" % q_
            ecum_, kec = ecum2[q_], "ecum%d" % q_
            kd = "dt%d_%d" % (p_, j)
            tt("dve", da, dtt[j], abc, ALU.mult, [kd, "abc"], ["da"])
            yield
            (pc, kc_), = yield from PSa(1)
            mm([(pc[:, 0:8], trif, da, True, True), (pc[:, 8:16], ones, da, True, True)], ["trif", "ones", "da"], [kc_])
            yield
            cp("dve", cumtot, pc[:, 0:16], [kc_], ["cumtot"])
            PSr(kc_)
            yield
            pR = yield from PSa(2)
            for hb in range(2):
                mm([(pR[hb][0][:, r * 128:(r + 1) * 128], da[:, hb * 4 + r:hb * 4 + r + 1].to_broadcast([128, 128]), trif,
                     True, True) for r in range(4)], ["da", "trif"], [pR[hb][1]])
                yield
            (pb, kb), = yield from PSa(1)
            pbb = pb.bitcast(BF16)
            tr([(pbb[:, c * 128:(c + 1) * 128], xact[:, c, tok]) for c in range(6)], identb, XACT + ["identb"], [kb])
            yield
            act(ecum_, cumtot, AF.Exp, ["cumtot"], [kec])
            yield
            tt("dve", dte, cumtot[:, 8:16], cumtot[:, 0:8], ALU.subtract, ["cumtot"], ["dte"])
            yield
            act(dte, dte, AF.Exp, ["dte"], ["dte"])
            yield
            cp("act", xsB_, pbb[:, 0:768], [kb], [kxs])
            PSr(kb)
            yield
            xs3 = xsB_[:, 0:512].rearrange("p (h d) -> p h d", h=8)
            tt("dve", dtd, dtt[j], dte, ALU.mult, [kd, "dte"], ["dtd"])
            yield
            for h in range(8):
                ts("dve", segc[:, h, :], pR[h // 4][0][:, (h % 4) * 128:(h % 4 + 1) * 128], cumtot[:, h:h + 1], 0.0,
                   ALU.subtract, ALU.min, [pR[h // 4][1], "cumtot"], ["segc"])
                yield
            PSr(pR[0][1], pR[1][1])
            act(segc, segc, AF.Exp, ["segc"], ["segc"])
            yield
            (pcb, kcb), = yield from PSa(1)
            mm([(pcb[:, g * 128:(g + 1) * 128], xact[:, 4 + g, tok], xact[:, 6 + g, tok], True, True) for g in range(2)],
               XACT, [kcb])
            yield
            tt("dve", cbm, pcb[:, 0:256].rearrange("p (g l) -> p g l", g=2),
               trif.unsqueeze(1).to_broadcast([128, 2, 128]), ALU.mult, [kcb, "trif"], ["cbm"])
            PSr(kcb)
            yield
            tt("dve", xc, xs3, dtt[j].unsqueeze(2).to_broadcast([128, 8, 64]), ALU.mult, [kxs, kd], ["xc"])
            yield
            tt("pool", xdte_, xs3, dtd.unsqueeze(2).to_broadcast([128, 8, 64]), ALU.mult, [kxs, "dtd"], [kxd])
            yield
            for g in range(2):
                tt("dve", MT[:, g * 4:(g + 1) * 4, :], segc[:, g * 4:(g + 1) * 4, :],
                   cbm[:, g, :].unsqueeze(1).to_broadcast([128, 4, 128]), ALU.mult, ["segc", "cbm"], ["MT"])
                yield
            (pyd, kyd), = yield from PSa(1)
            mm([(pyd[:, h * 64:(h + 1) * 64], MT[:, h, :], xc[:, h, :], True, True) for h in range(8)],
               ["MT", "xc"], [kyd])
            PYD[n] = (pyd, kyd)
            FL[("ssdF", n)] = True
            yield

    def ssdT_gen(s_):
        p_ = s_ % 2
        xact, zs = xacts[p_], zss[p_]
        XACT = ["xact%d_%d" % (p_, c) for c in range(8)]
        for j in range(4):
            n = s_ * 4 + j
            q_ = n % 2
            yield from wait(("ssdF", n))
            tok = slice(j * 128, (j + 1) * 128)
            xsB_, kxs = xsB2[q_], "xsB%d" % q_
            xdte_, kxd = xdte2[q_], "xdte%d" % q_
            ecum_, kec = ecum2[q_], "ecum%d" % q_
            xs3 = xsB_[:, 0:512].rearrange("p (h d) -> p h d", h=8)
            pyd, kyd = PYD.pop(n)
            (pyo, kyo), (pst_, kst) = yield from PSa(2)
            mm([(pyo[:, g * 256:(g + 1) * 256], xact[:, 6 + g, tok],
                 Sbf[:, g * 4:(g + 1) * 4, :].rearrange("p h d -> p (h d)"), True, True) for g in range(2)],
               XACT + ["Sbf"], [kyo])
            yield
            mm([(pst_[:, g * 256:(g + 1) * 256], xsB_[:, 512 + g * 128:512 + (g + 1) * 128],
                 xdte_[:, g * 4:(g + 1) * 4, :].rearrange("p h d -> p (h d)"), True, True) for g in range(2)],
               [kxs, kxd], [kst])
            yield
            tt("pool", y2, xs3, rows[:, R_DSK:R_DSK + 8].unsqueeze(2).to_broadcast([128, 8, 64]), ALU.mult,
               [kxs, "rows"], ["y2"])
            yield
            tt("dve", y1, pyo.rearrange("p (h d) -> p h d", h=8), ecum_[:, 0:8].unsqueeze(2).to_broadcast([128, 8, 64]),
               ALU.mult, [kyo, kec], ["y1"])
            yield
            tt("dve", y1, y1, pyd.rearrange("p (h d) -> p h d", h=8), ALU.add, ["y1", kyd], ["y1"])
            PSr(kyo, kyd)
            yield
            tt("dve", Sst, Sst, ecum_[:, 8:16].unsqueeze(2).to_broadcast([128, 8, 64]), ALU.mult, ["Sst", kec], ["Sst"])
            yield
            tt("dve", Sst, Sst, pst_.rearrange("p (h d) -> p h d", h=8), ALU.add, ["Sst", kst], ["Sst"])
            PSr(kst)
            yield
            cp("act", Sbf, Sst, ["Sst"], ["Sbf"])
            yield
            tt("dve", y1, y1, y2, ALU.add, ["y1", "y2"], ["y1"])
            yield
            y1f = y1.rearrange("p h d -> p (h d)")
            tt("dve", y1f, y1f, zs[j], ALU.mult, ["y1", "zs%d_%d" % (p_, j)], ["y1"])
            yield
            for g in range(2):
                act(junk[:, 0:256], y1f[:, g * 256:(g + 1) * 256], AF.Square, ["y1"], ["junk", "sdss"],
                    accum_out=smA[:, 12 + g:13 + g])
                yield
            yield from rstd_g(smA[:, 12:14], smA[:, 14:16], 1.0 / 256, "sdss", "sdrs")
            yield from wait(("op", n - 2))
            for g in range(2):
                stt("dve", mix[j % 2][:, 512 + g * 256:512 + (g + 1) * 256], y1f[:, g * 256:(g + 1) * 256],
                    smA[:, 14 + g:15 + g], rows[:, R_SNW + g * 256:R_SNW + (g + 1) * 256], ALU.mult, ALU.mult,
                    ["y1", "sdrs", "rows"], ["mixB%d" % (j % 2)])
                yield
            FL[("ssd_c", n)] = True
            FL[("ssdT", n)] = True
        FL[("ssd", s_)] = True

    def op_gen(s_):
        for j in range(4):
            n = s_ * 4 + j
            kx = "xrs0"
            yield from wait(("swa_c", n), ("ssd_c", n))
            S.dma("sp", xrs[0], x_d[n * 128:(n + 1) * 128, :], kx, writes=[kx])
            (pb, kb), = yield from PSa(1)
            pbb = pb.bitcast(BF16)
            mx_ = mix[j % 2]
            tr([(pbb[:, c * 128:(c + 1) * 128], mx_[:, c * 128:(c + 1) * 128]) for c in range(8)], identb,
               ["mixA%d" % (j % 2), "mixB%d" % (j % 2), "identb"], [kb])
            FL[("op", n)] = True
            yield
            cp("act", mixT.rearrange("p c t -> p (c t)"), pbb, [kb], ["mixT"])
            PSr(kb)
            yield
            for hf in range(2):
                (po, ko), = yield from PSa(1)
                mm([(po, mixT[:, kc, :], Wo[:, kc, hf * 512:(hf + 1) * 512], kc == 0, kc == 7) for kc in range(8)],
                   ["mixT", "Wo"], [ko])
                yield
                tt("dve", xrs[0][:, hf * 512:(hf + 1) * 512], po, xrs[0][:, hf * 512:(hf + 1) * 512], ALU.add,
                   [ko, kx], [kx])
                PSr(ko)
                yield
            S.dma("sp", x2_d[n * 128:(n + 1) * 128, :], xrs[0], kx, reads=[kx], writes=["x2d%d" % n])
            yield

    for k_ in ("swa", "ssd", "op", "ssdT", "rope"):
        FL[(k_, -1)] = True
        FL[(k_, -2)] = True
    run_streams([a1_gen(0, 0), a1_gen(0, 1), trig_gen(0)])
    g0 = [rope_gen(0), conv_gen(0, 0), conv_gen(0, 1), a3_gen(0), memkv_gen()]
    if NST > 1:
        g0 += [a1_gen(1, 0), a1_gen(1, 1), trig_gen(1)]
    run_streams(g0)
    S.dma("pool", Wo, wo_d.rearrange("(c p) n -> p c n", p=128), "Wo", writes=["Wo"])
    for st in range(NST):
        gens = [swa_gen(st), ssdF_gen(st), ssdT_gen(st), op_gen(st)]
        if st + 1 < NST:
            gens += [rope_gen(st + 1), conv_gen(st + 1, 0), conv_gen(st + 1, 1), a3_gen(st + 1)]
        if st + 2 < NST:
            gens += [a1_gen(st + 2, 0), a1_gen(st + 2, 1), trig_gen(st + 2)]
        run_streams(gens)
        chk(8)

    chk(9)
    A_keys = list(S.last_w.keys())
    A.off = mark0
    S.fence(A_keys)

    yacc = A.alloc([NPC, 1024], F32)
    x3nT = A.alloc([8, TP], BF16)
    gates = A.alloc([NPC, 32], F32)
    rstdm = A.alloc([NPC], F32)
    outb = [A.alloc([1024], F32) for _ in range(2)]
    smB = A.alloc([32], F32)
    Wxq = A.alloc([8, 512], BF16)
    Wxo = A.alloc([4, 1024], BF16)
    Wg0 = A.alloc([8, 256], BF16)
    Wu0 = A.alloc([8, 256], BF16)
    Wd0 = A.alloc([2, 1024], BF16)
    mark1 = A.off
    S.dma("pool", Wxq, xq_d.rearrange("(c p) n -> p c n", p=128), "Wxq", writes=["Wxq"])
    S.dma("pool", Wxo, xo_d.rearrange("(c p) n -> p c n", p=128), "Wxo", writes=["Wxo"])

    def load_w0():
        S.dma("pool", Wg0, wg_d[0].rearrange("(c p) f -> p c f", p=128), "We0", writes=["We0"])
        S.dma("pool", Wu0, wu_d[0].rearrange("(c p) f -> p c f", p=128), "We0", writes=["We0"])
        S.dma("pool", Wd0, wd_d[0].rearrange("(c p) n -> p c n", p=128), "We0", writes=["We0"])
    NSTP = NPC // 4

    FN = {}

    def fn_gen(pp):
        ssc, rsc = smB[:, 16:17], smB[:, 17:18]
        for ci in range(NPC):
            n = pp * NPC + ci
            ky = "y%d" % ci
            act(junk, yacc[:, ci, :], AF.Square, [ky], ["junk", "fnss"], accum_out=ssc)
            yield
            yield from rstd_g(ssc, rsc, 1.0 / D, "fnss", "fnrs")
            kob = "outb%d" % (ci % 2)
            stt("dve", outb[ci % 2], yacc[:, ci, :], rsc, rows[:, R_FNW:R_FNW + 1024], ALU.mult, ALU.mult,
                [ky, "fnrs", "rows"], [kob])
            FN[(pp, ci)] = True
            yield
            S.dma("sp", out_d[n * 128:(n + 1) * 128, :], outb[ci % 2], kob, reads=[kob])
            yield

    for p in range(NPASS):
        A.off = mark1
        if p == 0:
            S.fence(list(S.last_w.keys()))
        else:
            S.fence(["We1", "sg0", "sg1", "hid0", "hid1"])
        Wr32 = A.alloc([8, 36], F32)
        x2n = [A.alloc([1024], BF16) for _ in range(2)]
        x3n = [A.alloc([1024], BF16) for _ in range(2)]
        x2nT = A.alloc([8, 512], BF16)
        qxT = A.alloc([4, 512], BF16)
        PxT = [A.alloc([512], BF16) for _ in range(4)]
        ob = [A.alloc([4, 512], BF16) for _ in range(2)]
        oT = [A.alloc([4, 128], BF16) for _ in range(2)]
        x3T32 = [A.alloc([8, 128], F32) for _ in range(2)]
        lgs = [A.alloc([36], F32) for _ in range(2)]
        rsms = [A.alloc([32], F32) for _ in range(2)]
        msks = [A.alloc([32], F32) for _ in range(2)]
        g1s = [A.alloc([32], F32) for _ in range(2)]
        top8s = [A.alloc([8], F32) for _ in range(2)]
        load_w0()
        S.dma("sp", Wr32, wr_d.rearrange("(c p) n -> p c n", p=128), "Wr32", writes=["Wr32"])
        tt("dve", Wr32, Wr32, cols[:, C_FFNW:C_FFNW + 8].unsqueeze(2).to_broadcast([128, 8, 36]), ALU.mult,
           ["Wr32", "cols"], ["Wr32"])
        FB = {}

        def waitb(*ks):
            while not all(FB.get(k) for k in ks):
                yield

        def la_gen(par, p=p):
            ssc, rsc = smB[:, par * 2:par * 2 + 1], smB[:, par * 2 + 1:par * 2 + 2]
            kss, krs, kxn = "lass%d" % par, "lars%d" % par, "x2n%d" % par
            for s_ in range(NSTP):
                for j in (par, par + 2):
                    ci = s_ * 4 + j
                    n = p * NPC + ci
                    ky = "y%d" % ci
                    while p > 0 and not FN.get((p - 1, ci)):
                        yield
                    S.dma("sp", yacc[:, ci, :], x2_d[n * 128:(n + 1) * 128, :], ky, reads=["x2d%d" % n], writes=[ky])
                    yield
                    act(junk, yacc[:, ci, :], AF.Square, [ky], ["junk", kss], accum_out=ssc)
                    yield
                    yield from rstd_g(ssc, rsc, 1.0 / D, kss, krs)
                    act(x2n[par], yacc[:, ci, :], AF.Copy, [ky, krs], [kxn], scale=rsc)
                    yield
                    yield from waitb(("qx", s_ - 1))
                    (pb, kb), = yield from PSa(1)
                    pbb = pb.bitcast(BF16)
                    tr([(pbb[:, c * 128:(c + 1) * 128], x2n[par][:, c * 128:(c + 1) * 128]) for c in range(8)], identb,
                       [kxn, "identb"], [kb])
                    yield
                    tt("dve", x2nT[:, :, j * 128:(j + 1) * 128], pbb.rearrange("p (c t) -> p c t", c=8),
                       cols[:, C_XAW:C_XAW + 8].unsqueeze(2).to_broadcast([128, 8, 128]), ALU.mult,
                       [kb, "cols"], ["x2nT%d" % j])
                    PSr(kb)
                    yield
                FB[("la", s_, par)] = True

        X2 = ["x2nT%d" % j for j in range(4)]

        def xa_gen():
            for s_ in range(NSTP):
                yield from waitb(("la", s_, 0), ("la", s_, 1))
                for h in range(4):
                    (pb, kb), = yield from PSa(1)
                    mm([(pb, Wxq[:, kc, h * 128:(h + 1) * 128], x2nT[:, kc, :], kc == 0, kc == 7) for kc in range(8)],
                       ["Wxq"] + X2, [kb])
                    yield
                    act(qxT[:, h, :], pb, AF.Copy, [kb], ["qxT"])
                    PSr(kb)
                    yield
                FB[("qx", s_)] = True
                yield from waitb(("xo", s_ - 2, 0), ("xo", s_ - 2, 1))
                obs = ob[s_ % 2]
                for h in range(4):
                    for m in range(2):
                        (pb, kb), = yield from PSa(1)
                        mm([(pb, KmT[:, h, m * 128:(m + 1) * 128], qxT[:, h, :], True, True)], ["KmT", "qxT"], [kb])
                        yield
                        act(PxT[(h % 2) * 2 + m], pb, AF.Exp, [kb], ["PxT%d" % ((h % 2) * 2 + m)], scale=float(128 ** -0.5))
                        PSr(kb)
                        yield
                    for j in range(4):
                        (pO, kO), = yield from PSa(1)
                        mm([(pO[:, 0:129], PxT[(h % 2) * 2 + m][:, j * 128:(j + 1) * 128], Vm[:, m, h, :], m == 0, m == 1)
                            for m in range(2)], ["PxT%d" % ((h % 2) * 2), "PxT%d" % ((h % 2) * 2 + 1), "Vm"], [kO])
                        yield
                        recip(smB[:, 8:9], pO[:, 128:129], [kO], ["rd"])
                        yield
                        ts("dve", obs[:, j, h * 128:(h + 1) * 128], pO[:, 0:128], smB[:, 8:9], None, ALU.mult, None,
                           [kO, "rd"], ["ob%d_%d" % (s_ % 2, j)])
                        PSr(kO)
                        yield
                FB[("xa", s_)] = True

        def xo_gen(par):
            ssc = smB[:, 12 + par:13 + par]
            kss = "xoss%d" % par
            lg, rsm, msk, g1, top8 = lgs[par], rsms[par], msks[par], g1s[par], top8s[par]
            klg, krsm, kmsk, kg1, kt8 = "lg%d" % par, "rsm%d" % par, "msk%d" % par, "g1%d" % par, "t8%d" % par
            for s_ in range(NSTP):
                yield from waitb(("xa", s_))
                obs = ob[s_ % 2]
                for j in (par, par + 2):
                    ci = s_ * 4 + j
                    ky = "y%d" % ci
                    kob = "ob%d_%d" % (s_ % 2, j)
                    koT, kx3, k32 = "oT%d" % par, "x3n%d" % par, "x3T%d" % par
                    (pb, kb), = yield from PSa(1)
                    pbb = pb.bitcast(BF16)
                    tr([(pbb[:, c * 128:(c + 1) * 128], obs[:, j, c * 128:(c + 1) * 128]) for c in range(4)], identb,
                       [kob, "identb"], [kb])
                    yield
                    cp("act", oT[par].rearrange("p c t -> p (c t)"), pbb[:, 0:512], [kb], [koT])
                    PSr(kb)
                    yield
                    for hf in range(2):
                        (po, ko), = yield from PSa(1)
                        mm([(po, oT[par][:, kc, :], Wxo[:, kc, hf * 512:(hf + 1) * 512], kc == 0, kc == 3)
                            for kc in range(4)], [koT, "Wxo"], [ko])
                        yield
                        tt("dve", yacc[:, ci, hf * 512:(hf + 1) * 512], po, yacc[:, ci, hf * 512:(hf + 1) * 512], ALU.add,
                           [ko, ky], [ky])
                        PSr(ko)
                        yield
                    act(junk, yacc[:, ci, :], AF.Square, [ky], ["junk", kss], accum_out=ssc)
                    yield
                    yield from rstd_g(ssc, rstdm[:, ci:ci + 1], 1.0 / D, kss, "rstdm%d" % ci)
                    krm = "rstdm%d" % ci
                    act(x3n[par], yacc[:, ci, :], AF.Copy, [ky, krm], [kx3], scale=rstdm[:, ci:ci + 1])
                    yield
                    (pb, kb), = yield from PSa(1)
                    pbb = pb.bitcast(BF16)
                    tr([(pbb[:, c * 128:(c + 1) * 128], x3n[par][:, c * 128:(c + 1) * 128]) for c in range(8)], identb,
                       [kx3, "identb"], [kb])
                    yield
                    tt("dve", x3nT[:, :, ci * 128:(ci + 1) * 128], pbb.rearrange("p (c t) -> p c t", c=8),
                       cols[:, C_FFNW:C_FFNW + 8].unsqueeze(2).to_broadcast([128, 8, 128]), ALU.mult,
                       [kb, "cols"], ["x3nT%d" % (ci // 4)])
                    PSr(kb)
                    yield
                    for hb in range(2):
                        (pb, kb), = yield from PSa(1)
                        tr([(pb[:, c * 128:(c + 1) * 128], yacc[:, ci, (hb * 4 + c) * 128:(hb * 4 + c + 1) * 128])
                            for c in range(4)], identf, [ky, "identf"], [kb])
                        yield
                        act(x3T32[par][:, hb * 4:(hb + 1) * 4, :].rearrange("p c t -> p (c t)"), pb, AF.Copy, [kb], [k32])
                        PSr(kb)
                        yield
                    (pl, kl), = yield from PSa(1)
                    mm([(pl[:, 0:36], x3T32[par][:, kc, :], Wr32[:, kc, :], kc == 0, kc == 7) for kc in range(8)],
                       [k32, "Wr32"], [kl])
                    yield
                    stt("dve", lg, pl[:, 0:36], rstdm[:, ci:ci + 1], rows[:, R_RB:R_RB + 36], ALU.mult, ALU.add,
                        [kl, krm, "rows"], [klg])
                    PSr(kl)
                    yield
                    S.op("dve", lambda e: e.tensor_reduce(out=rsm[:, 0:1], in_=lg[:, 0:4], axis=mybir.AxisListType.X,
                                                          op=ALU.max), [klg], [krsm])
                    yield
                    ts("dve", rsm[:, 1:2], rsm[:, 0:1], -1.0, None, ALU.mult, None, [krsm], [krsm])
                    yield
                    act(rsm[:, 4:8], lg[:, 0:4], AF.Exp, [klg, krsm], [krsm], bias=rsm[:, 1:2], accum_out=rsm[:, 2:3])
                    yield
                    recip(rsm[:, 3:4], rsm[:, 2:3], [krsm], [krsm])
                    yield
                    ts("dve", rsm[:, 8:12], lg[:, 0:4], rsm[:, 0:1], 1e30, ALU.is_ge, ALU.mult, [klg, krsm], [krsm])
                    yield
                    ts("dve", rsm[:, 8:12], rsm[:, 8:12], -1e30, None, ALU.add, None, [krsm], [krsm])
                    yield
                    tt("dve", msk.rearrange("p (g e) -> p g e", g=4), lg[:, 4:36].rearrange("p (g e) -> p g e", g=4),
                       rsm[:, 8:12].unsqueeze(2).to_broadcast([128, 4, 8]), ALU.add, [klg, krsm], [kmsk])
                    yield
                    S.op("dve", lambda e: e.max(out=top8, in_=msk), [kmsk], [kt8])
                    yield
                    tt("dve", rsm[:, 12:13], top8[:, 1:2], top8[:, 0:1], ALU.subtract, [kt8], [krsm])
                    yield
                    act(rsm[:, 12:13], rsm[:, 12:13], AF.Exp, [krsm], [krsm])
                    yield
                    ts("dve", rsm[:, 12:13], rsm[:, 12:13], 1.0, None, ALU.add, None, [krsm], [krsm])
                    yield
                    recip(rsm[:, 13:14], rsm[:, 12:13], [krsm], [krsm])
                    yield
                    tt("dve", rsm[:, 14:15], rsm[:, 13:14], rsm[:, 3:4], ALU.mult, [krsm], [krsm])
                    yield
                    tt("dve", rsm[:, 15:16], rsm[:, 3:4], rsm[:, 14:15], ALU.subtract, [krsm], [krsm])
                    yield
                    ts("dve", g1, msk, top8[:, 0:1], rsm[:, 14:15], ALU.is_equal, ALU.mult, [kmsk, kt8, krsm], [kg1])
                    yield
                    ts("dve", msk, msk, top8[:, 1:2], rsm[:, 15:16], ALU.is_equal, ALU.mult, [kmsk, kt8, krsm], [kmsk])
                    yield
                    tt("dve", gates[:, ci, :], g1, msk, ALU.add, [kg1, kmsk], ["gates"])
                    yield
                FB[("xo", s_, par)] = True

        FB[("qx", -1)] = True
        for q_ in (-1, -2):
            FB[("xo", q_, 0)] = True
            FB[("xo", q_, 1)] = True
        run_streams([la_gen(0), la_gen(1), xa_gen(), xo_gen(0), xo_gen(1)] + ([fn_gen(p - 1)] if p > 0 else []))
        chk(11)

        A.off = mark1
        S.fence(list(S.last_w.keys()))
        Wg = [Wg0, A.alloc([8, 256], BF16)]
        Wu = [Wu0, A.alloc([8, 256], BF16)]
        Wd = [Wd0, A.alloc([2, 1024], BF16)]
        sg = [A.alloc([512], F32) for _ in range(2)]
        hid = [A.alloc([2, 512], BF16) for _ in range(2)]
        hot = 0
        X3 = ["x3nT%d" % i for i in range(NPC // 4)]
        steps = [(e_, st) for e_ in range(32) for st in range(NSTP)]
        gu_i = [0]

        def PSg():
            i = gu_i[0] % 2
            gu_i[0] += 1
            return ((pst[:, (2 * i) * 512:(2 * i + 1) * 512], "ps%d" % (2 * i)),
                    (pst[:, (2 * i + 1) * 512:(2 * i + 2) * 512], "ps%d" % (2 * i + 1)))
        dn_i = [0]

        def PSd():
            i = 4 + dn_i[0] % 4
            dn_i[0] += 1
            return pst[:, i * 512:(i + 1) * 512], "ps%d" % i

        def load_w(e_):
            sl = e_ % 2
            kw = "We%d" % sl
            S.dma("pool", Wg[sl], wg_d[e_].rearrange("(c p) f -> p c f", p=128), kw, writes=[kw])
            S.dma("pool", Wu[sl], wu_d[e_].rearrange("(c p) f -> p c f", p=128), kw, writes=[kw])
            S.dma("pool", Wd[sl], wd_d[e_].rearrange("(c p) n -> p c n", p=128), kw, writes=[kw])

        def gu(step, f):
            e_, st = step
            sl = e_ % 2
            kw = "We%d" % sl
            stok = slice(st * 512, (st + 1) * 512)
            (pg_, kg_), (pu_, ku_) = PSg()
            mm([(pg_, Wg[sl][:, kc, f * 128:(f + 1) * 128], x3nT[:, kc, stok], kc == 0, kc == 7)
                for kc in range(8)], [kw, X3[st]], [kg_])
            mm([(pu_, Wu[sl][:, kc, f * 128:(f + 1) * 128], x3nT[:, kc, stok], kc == 0, kc == 7)
                for kc in range(8)], [kw, X3[st]], [ku_])
            return (pg_, kg_, pu_, ku_)

        pend = {0: gu(steps[0], 0)}
        for k, step in enumerate(steps):
            e_, st = step
            sl = e_ % 2
            kw = "We%d" % sl
            pend[1] = gu(step, 1)
            hh = hid[hot]
            kh = "hid%d" % hot
            hot ^= 1
            for f in range(2):
                pg_, kg_, pu_, ku_ = pend[f]
                act(sg[f], pg_, AF.Silu, [kg_], ["sg%d" % f])
                tt("dve", hh[:, f, :], sg[f], pu_, ALU.mult, ["sg%d" % f, ku_], [kh])
            if k + 1 < len(steps):
                if steps[k + 1][0] != e_:
                    load_w(steps[k + 1][0])
                pend[0] = gu(steps[k + 1], 0)
            for j in range(4):
                ci = st * 4 + j
                ky = "y%d" % ci
                for hf in range(2):
                    po, ko = PSd()
                    mm([(po, hh[:, f, j * 128:(j + 1) * 128], Wd[sl][:, f, hf * 512:(hf + 1) * 512], f == 0, f == 1)
                        for f in range(2)], [kh, kw], [ko])
                    stt("dve", yacc[:, ci, hf * 512:(hf + 1) * 512], po, gates[:, ci, e_:e_ + 1],
                        yacc[:, ci, hf * 512:(hf + 1) * 512], ALU.mult, ALU.add, [ko, "gates", ky], [ky])
        chk(12)
    run_streams([fn_gen(NPASS - 1)])
    S.finish("sp")
    return nc


def _prep_shared(inp):
    f32 = np.float32
    w_in = np.asarray(inp["w_in"], f32)[0]
    q = w_in[:, 0:512]
    k = w_in[:, 512:640]
    v = w_in[:, 640:768]
    z = w_in[:, 768:1280]
    xbc = w_in[:, 1280:2304]
    dt = w_in[:, 2304:2312]

    def rot(wc, nh):
        w3 = wc.reshape(D, nh, 64).copy()
        r = w3.copy()
        r[:, :, 0:8] = w3[:, :, 8:16]
        r[:, :, 8:16] = w3[:, :, 0:8]
        return r.reshape(D, nh * 64)

    qr = rot(q, 8)
    kr = rot(k, 2)
    kd = [np.concatenate([k[:, g * 64:(g + 1) * 64]] * 2, axis=1) for g in range(2)]
    krd = [np.concatenate([kr[:, g * 64:(g + 1) * 64]] * 2, axis=1) for g in range(2)]
    wf = np.ascontiguousarray(np.concatenate([q, kd[0], kd[1], xbc], axis=1))
    rotm = np.zeros((128, 128), f32)
    for m_ in range(128):
        d_ = m_ % 64
        k_ = m_ + 8 if d_ < 8 else (m_ - 8 if d_ < 16 else m_)
        rotm[k_, m_] = 1.0
    wt = np.ascontiguousarray(np.concatenate([z, v, dt], axis=1))
    assert wf.shape == (D, 1792) and wt.shape == (D, 648)

    def colv(vv):
        return np.asarray(vv, f32).reshape(8, 128).T

    cols = np.zeros((128, NCOL), f32)
    cols[:, C_MIXW:C_MIXW + 8] = colv(inp["mix_norm_w"][0])
    cols[:, C_XAW:C_XAW + 8] = colv(inp["xattn_norm_w"][0])
    cols[:, C_MEMW:C_MEMW + 8] = colv(inp["mem_norm_w"][0])
    cols[:, C_FFNW:C_FFNW + 8] = colv(inp["ffn_norm_w"][0])
    cols[:, C_CONVB:C_CONVB + 8] = colv(inp["ssd_conv_b"][0])
    cw = np.asarray(inp["ssd_conv_w"], f32)[0]
    for c in range(8):
        for j in range(4):
            cols[:, C_CONVW + c * 4 + j] = cw[j, c * 128:(c + 1) * 128]
    inv = (np.float32(500000.0) ** (-np.arange(0, 16, 2, dtype=np.float32) / np.float32(16))).astype(f32)
    invf = np.zeros(128, f32)
    for p_ in range(128):
        d_ = p_ % 64
        if d_ < 8:
            invf[p_] = -inv[d_]
        elif d_ < 16:
            invf[p_] = inv[d_ - 8]
    cols[:, C_INVF] = invf

    rows1 = np.zeros(NROW, f32)
    rows1[R_SINK:R_SINK + 8] = inp["attn_sinks"][0]
    rows1[R_DTB:R_DTB + 8] = inp["ssd_dt_bias"][0]
    rows1[R_ALOG:R_ALOG + 8] = inp["ssd_a_log"][0]
    rows1[R_DSK:R_DSK + 8] = inp["ssd_d"][0]
    rows1[R_RB:R_RB + 4] = inp["router_group_b"][0]
    rows1[R_RB + 4:R_RB + 36] = inp["router_expert_b"][0]
    rows1[R_ANW:R_ANW + 512] = inp["attn_out_norm_w"][0]
    rows1[R_SNW:R_SNW + 512] = inp["ssd_out_norm_w"][0]
    rows1[R_FNW:R_FNW + 1024] = inp["final_norm_w"]
    rows = np.ascontiguousarray(np.broadcast_to(rows1, (128, NROW)))
    wr = np.ascontiguousarray(np.concatenate([np.asarray(inp["router_group_w"], f32)[0],
                                              np.asarray(inp["router_expert_w"], f32)[0]], axis=1))
    return {
        "wf": wf, "wt": wt, "wo": np.ascontiguousarray(np.asarray(inp["w_out"], f32)[0]),
        "xq": np.ascontiguousarray(np.asarray(inp["xattn_w_q"], f32)[0]),
        "xkv": np.ascontiguousarray(np.asarray(inp["xattn_w_kv"], f32)[0]),
        "xo": np.ascontiguousarray(np.asarray(inp["xattn_w_o"], f32)[0]),
        "wr": wr,
        "wg": np.ascontiguousarray(np.asarray(inp["expert_w_gate"], f32)[0]),
        "wu": np.ascontiguousarray(np.asarray(inp["expert_w_up"], f32)[0]),
        "wd": np.ascontiguousarray(np.asarray(inp["expert_w_down"], f32)[0]),
        "cols": cols, "rows": rows,
        "ident": np.eye(128, dtype=f32), "tri": np.triu(np.ones((128, 128), f32)), "rotm": rotm,
    }


def run(inp, T, stop=99):
    shared = _prep_shared(inp)
    nc = build(T, stop)
    in_maps = []
    for b in range(NCORES):
        m = dict(shared)
        m["x"] = np.ascontiguousarray(np.asarray(inp["x"], np.float32)[b, :T])
        m["mem"] = np.ascontiguousarray(np.asarray(inp["mem"], np.float32)[b])
        m["pos"] = np.ascontiguousarray(np.broadcast_to(np.asarray(inp["positions"], np.int32)[b, :T][None, :], (128, T)))
        in_maps.append(m)
    res = run_bass_kernel_spmd(nc, in_maps, core_ids=list(range(NCORES)))
    return np.stack([np.asarray(r["out"], np.float32) for r in res.results], axis=0)


def kernel(**inputs):
    return run(inputs, SEQ)
```

```python
import numpy as np
import concourse.bass as bass
import concourse.mybir as mybir
from concourse.bass_utils import run_bass_kernel_spmd

F32 = mybir.dt.float32
BF16 = mybir.dt.bfloat16
I32 = mybir.dt.int32
AF = mybir.ActivationFunctionType
ALU = mybir.AluOpType
PI = float(np.pi)

D = 1024
NCORES = 8
SEQ = 4096
MEMT = 256
C_MIXW, C_XAW, C_MEMW, C_FFNW, C_CONVB, C_CONVW, C_INVF, NCOL = 0, 8, 16, 24, 32, 40, 72, 73
R_SINK, R_DTB, R_ALOG, R_DSK, R_RB, R_ANW, R_SNW, R_FNW, NROW = 0, 8, 16, 24, 32, 68, 580, 1092, 2116


class Sched:
    def __init__(self, nc):
        self.nc = nc
        self.eng = {"pe": nc.tensor, "act": nc.scalar, "dve": nc.vector,
                    "pool": nc.gpsimd, "sp": nc.sync}
        self.sem = {}
        self.cnt = {}
        for e in self.eng:
            self.sem[e] = nc.alloc_semaphore("s_" + e)
            self.cnt[e] = 0
        self.waited = {e: {} for e in self.eng}
        self.last_w = {}
        self.readers = {}
        self.dsem = {}
        self.dcnt = {}
        self.defer = None
        self.vc = {}
        self.efree = {e: 0.0 for e in self.eng}
        self.kready = {}
        self.kread_t = {}
        self.last_finish = 0.0

    def _model(self, e, reads, writes, dur, dma=False, occ=0.1):
        lat = 0.35
        t = self.efree[e]
        for b in list(reads) + list(writes):
            t = max(t, self.kready.get(b, 0.0) + lat)
        for b in writes:
            t = max(t, self.kread_t.get(b, 0.0) + lat)
        fin = t + dur
        self.efree[e] = t + occ if dma else fin
        for b in writes:
            self.kready[b] = fin
            self.kread_t[b] = 0.0
        for b in reads:
            self.kread_t[b] = max(self.kread_t.get(b, 0.0), fin)
        self.last_finish = fin

    def _need(self, e, ev):
        if ev is None:
            return
        k, v = ev
        if self.waited[e].get(k, 0) >= v:
            return
        self.waited[e][k] = v
        for k2, v2 in self.vc.get((k, v), {}).items():
            if self.waited[e].get(k2, 0) < v2:
                self.waited[e][k2] = v2
        s = self.sem[k] if k in self.sem else self.dsem[k]
        if self.defer is not None:
            self.defer[k] = (s, v)
        else:
            self.eng[e].wait_ge(s, v)

    def _deps(self, e, reads, writes):
        for b in list(reads) + list(writes):
            self._need(e, self.last_w.get(b))
        for b in list(writes) + [r for r in reads if isinstance(r, str) and r.startswith("ps")]:
            for ev in self.readers.get(b, ()):
                if b in writes or ev[0] != e:
                    self._need(e, ev)

    def _record(self, ev, reads, writes):
        for b in writes:
            self.last_w[b] = ev
            self.readers[b] = []
        for b in reads:
            d = dict(self.readers.get(b, []))
            d[ev[0]] = max(d.get(ev[0], 0), ev[1])
            self.readers[b] = list(d.items())

    def op(self, e, fn, reads=(), writes=(), cost=0.5):
        self._model(e, reads, writes, cost)
        self.defer = {}
        self._deps(e, reads, writes)
        waits = list(self.defer.values())
        self.defer = None
        for (s_, v_) in waits[:-1]:
            self.eng[e].wait_ge(s_, v_)
        ins = fn(self.eng[e])
        first = ins
        if isinstance(ins, tuple):
            first, ins = ins
        if waits:
            first._wait_ge(*waits[-1])
        self.cnt[e] += 1
        ins.then_inc(self.sem[e], 1)
        vc_ = dict(self.waited[e])
        vc_[e] = max(vc_.get(e, 0), self.cnt[e] - 1)
        self.vc[(e, self.cnt[e])] = vc_
        self._record((e, self.cnt[e]), reads, writes)

    def dma(self, q, out, in_, key, reads=(), writes=()):
        nb = 1
        for d_ in out.shape:
            nb *= int(d_)
        nb *= 2 if out.dtype == BF16 else 4
        self._model(q, reads, writes, 2.0 + nb / 150e3, dma=True, occ=(nb / 175e3 if q == "pool" else 0.1))
        dk = ("d", key)
        if dk not in self.dsem:
            self.dsem[dk] = self.nc.alloc_semaphore("d_%d" % len(self.dsem))
            self.dcnt[dk] = 0
        self._deps(q, reads, writes)
        ins = self.eng[q].dma_start(out=out, in_=in_)
        self.dcnt[dk] += 16
        ins.then_inc(self.dsem[dk], 16)
        self.vc[(dk, self.dcnt[dk])] = dict(self.waited[q])
        self._record((dk, self.dcnt[dk]), reads, writes)

    def fence(self, keys):
        for e in self.eng:
            for b in keys:
                self._need(e, self.last_w.get(b))
                for ev in self.readers.get(b, ()):
                    self._need(e, ev)

    def finish(self, q="sp"):
        for e in self.eng:
            if self.cnt[e] > 0:
                self._need(q, (e, self.cnt[e]))
        for dk, v in self.dcnt.items():
            if v > 0:
                self._need(q, (dk, v))


class Arena:
    def view(self, off, shape, dtype):
        save = self.off
        self.off = off
        a = self.alloc(shape, dtype)
        self.off = save
        return a

    def __init__(self, nc, nbytes):
        self.t = nc.alloc_sbuf_tensor("arena", [128, nbytes // 4], F32)
        self.cap = nbytes
        self.off = 0

    def alloc(self, shape, dtype):
        esz = 2 if dtype == BF16 else 4
        n = int(np.prod(shape))
        nb = (n * esz + 31) // 32 * 32
        assert self.off + nb <= self.cap, ("SBUF arena overflow", self.off, nb, self.cap)
        a = self.t[:, self.off // 4:(self.off + nb) // 4]
        self.off += nb
        if dtype != F32:
            a = a.bitcast(dtype)
        a = a[:, 0:n]
        if len(shape) == 2:
            a = a.rearrange("p (a b) -> p a b", a=shape[0])
        elif len(shape) == 3:
            a = a.rearrange("p (a b c) -> p a b c", a=shape[0], b=shape[1])
        return a


def build(T, stop=99):
    class _Stop(Exception):
        pass

    def chk(stage):
        if stop <= stage:
            raise _Stop()

    try:
        return _build(T, chk)
    except _Stop:
        pass
    return _NC[0]


_NC = [None]


def _build(T, chk):
    assert T % 512 == 0
    NCH = T // 128
    NST = T // 512
    TP = min(T, 2048)
    NPASS = T // TP
    NPC = TP // 128
    nc = bass.Bass("TRN2", target_bir_lowering=False)

    def din(name, shape, dt=F32):
        return nc.dram_tensor(name, list(shape), dt, kind="ExternalInput").ap()

    x_d = din("x", [T, D])
    mem_d = din("mem", [MEMT, D])
    pos_d = din("pos", [128, T], I32)
    wf_d = din("wf", [D, 1792])
    wt_d = din("wt", [D, 648])
    wo_d = din("wo", [D, D])
    xq_d = din("xq", [D, 512])
    xkv_d = din("xkv", [D, 1024])
    xo_d = din("xo", [512, D])
    wr_d = din("wr", [D, 36])
    wg_d = din("wg", [32, D, 256])
    wu_d = din("wu", [32, D, 256])
    wd_d = din("wd", [32, 256, D])
    cols_d = din("cols", [128, NCOL])
    rows_d = din("rows", [128, NROW])
    ident_d = din("ident", [128, 128])
    tri_d = din("tri", [128, 128])
    rotm_d = din("rotm", [128, 128])
    out_d = nc.dram_tensor("out", [T, D], F32, kind="ExternalOutput").ap()
    x2_d = nc.dram_tensor("x2scr", [T, D], F32).ap()

    _NC[0] = nc
    S = Sched(nc)
    A = Arena(nc, 206 * 1024)
    _chk = chk

    def chk(stage):
        try:
            _chk(stage)
        except Exception:
            S.finish("sp")
            raise
    pst = nc.alloc_psum_tensor("pst", [128, 4096], F32)
    ps_i = [0]

    ps_busy = [False] * 8

    def PS():
        for _ in range(8):
            i = ps_i[0]
            ps_i[0] = (i + 1) % 8
            if not ps_busy[i]:
                return pst[:, i * 512:(i + 1) * 512], "ps%d" % i
        raise RuntimeError("no free PSUM bank")

    def PSa(n=1):
        while True:
            free = [(ps_i[0] + d) % 8 for d in range(8) if not ps_busy[(ps_i[0] + d) % 8]]
            if len(free) >= n:
                take = free[:n]
                for i in take:
                    ps_busy[i] = True
                ps_i[0] = (take[-1] + 1) % 8
                return [(pst[:, i * 512:(i + 1) * 512], "ps%d" % i) for i in take]
            yield

    def PSr(*keys):
        for k in keys:
            ps_busy[int(k[2:])] = False

    def fsz(ap):
        n = 1
        for d in ap.shape[1:]:
            n *= int(d)
        return n

    def ecost(eng, out):
        n = fsz(out)
        if eng == "act":
            return 0.28 + n / 1200.0
        if eng == "pool":
            return 0.15 + n / 450.0
        return 0.07 + n / 960.0

    def act(out, in_, func, R, W, **kw):
        S.op("act", lambda e: e.activation(out=out, in_=in_, func=func, **kw), R, W, cost=ecost("act", out))

    def tt(eng, out, in0, in1, op, R, W):
        S.op(eng, lambda e: e.tensor_tensor(out=out, in0=in0, in1=in1, op=op), R, W, cost=ecost(eng, out))

    def ts(eng, out, in0, s1, s2, op0, op1, R, W):
        if op1 is None:
            S.op(eng, lambda e: e.tensor_scalar(out=out, in0=in0, scalar1=s1, scalar2=None, op0=op0), R, W,
                 cost=ecost(eng, out))
        else:
            S.op(eng, lambda e: e.tensor_scalar(out=out, in0=in0, scalar1=s1, scalar2=s2, op0=op0, op1=op1), R, W,
                 cost=ecost(eng, out))

    def stt(eng, out, in0, sc, in1, op0, op1, R, W):
        S.op(eng, lambda e: e.scalar_tensor_tensor(out=out, in0=in0, scalar=sc, in1=in1, op0=op0, op1=op1), R, W,
             cost=ecost(eng, out))

    def cp(eng, out, in_, R, W):
        if eng == "act":
            S.op(eng, lambda e: e.activation(out=out, in_=in_, func=AF.Copy), R, W, cost=ecost(eng, out))
        else:
            S.op(eng, lambda e: e.tensor_copy(out=out, in_=in_), R, W, cost=ecost(eng, out))

    def mm(lst, R, W):
        def f(e):
            first = None
            for (o, l, r, st, sp) in lst:
                i = e.matmul(o, lhsT=l, rhs=r, start=st, stop=sp)
                first = first or i
            return (first, i)
        c = sum(max(0.06, fsz(r) / 2400.0 + 0.005) * (4.0 if l.dtype == F32 else 1.0) for (o, l, r, st, sp) in lst)
        S.op("pe", f, R, W, cost=c)

    def tr(lst, idn, R, W):
        def f(e):
            first = None
            for (o, i_) in lst:
                i = e.transpose(out=o, in_=i_, identity=idn)
                first = first or i
            return (first, i)
        S.op("pe", f, R, W, cost=len(lst) * (0.3 if idn.dtype == F32 else 0.1))

    def recip(out, in_, R, W):
        S.op("dve", lambda e: e.reciprocal(out=out, in_=in_), R, W)

    def memset(eng, ap, v, W):
        S.op(eng, lambda e: e.memset(ap, v), (), W)

    cols = A.alloc([NCOL], F32)
    rows = A.alloc([NROW], F32)
    identf = A.alloc([128], F32)
    identb = A.alloc([128], BF16)
    trif = A.alloc([128], F32)
    trib = A.alloc([128], BF16)
    ntrib = A.alloc([128], BF16)
    ones = A.alloc([128], F32)
    esink = A.alloc([8], F32)
    abc = A.alloc([8], F32)
    junk = A.alloc([1024], F32)
    sm = A.alloc([64], F32)
    KmT = A.alloc([4, 256], BF16)
    Vm = A.alloc([2, 4, 129], BF16)

    S.dma("sp", cols, cols_d, "cols", writes=["cols"])
    S.dma("sp", rows, rows_d, "rows", writes=["rows"])
    S.dma("sp", identf, ident_d, "identf", writes=["identf"])
    S.dma("sp", trif, tri_d, "trif", writes=["trif"])
    cp("dve", identb, identf, ["identf"], ["identb"])
    cp("dve", trib, trif, ["trif"], ["trib"])
    ts("dve", ntrib, trif, -1.0, 1.0, ALU.mult, ALU.add, ["trif"], ["ntrib"])
    memset("dve", ones, 1.0, ["ones"])
    act(esink, rows[:, R_SINK:R_SINK + 8], AF.Exp, ["rows"], ["esink"])
    act(abc, rows[:, R_ALOG:R_ALOG + 8], AF.Exp, ["rows"], ["abc"])
    ts("dve", abc, abc, -1.0, None, ALU.mult, None, ["abc"], ["abc"])
    one_c = ones[:, 0:1]
    rotb = A.alloc([128], BF16)
    S.dma("sp", junk[:, 0:128], rotm_d, "junk", writes=["junk"])
    cp("dve", rotb, junk[:, 0:128], ["junk"], ["rotb"])
    mbias = [A.alloc([2, 128], BF16) for _ in range(2)]
    ts("dve", mbias[0], trif.unsqueeze(1).to_broadcast([128, 2, 128]), -30000.0, None, ALU.mult, None,
       ["trif"], ["mbias"])
    ts("dve", mbias[1], trif.unsqueeze(1).to_broadcast([128, 2, 128]), 30000.0, -30000.0, ALU.mult, ALU.add,
       ["trif"], ["mbias"])
    epsc = A.alloc([1], F32)
    memset("dve", epsc, 1e-5, ["epsc"])
    chk(1)

    def rstd_chain(ss, rstd, inv_n, kss, krs):
        act(rstd, ss, AF.Ln, [kss, "epsc"], [krs], scale=inv_n, bias=epsc)
        act(rstd, rstd, AF.Exp, [krs], [krs], scale=-0.5)

    mark0 = A.off

    Wf = A.alloc([8, 1792], BF16)
    Wt = A.alloc([8, 648], BF16)
    Wo = A.alloc([8, 1024], BF16)
    for kc in range(8):
        S.dma("pool", Wf[:, kc, :], wf_d[kc * 128:(kc + 1) * 128, :], "Wf", writes=["Wf"])
    S.dma("pool", Wt, wt_d.rearrange("(c p) n -> p c n", p=128), "Wt", writes=["Wt"])

    xin = [A.alloc([1024], F32) for _ in range(2)]
    xrs = [A.alloc([1024], F32)]
    xn = [A.alloc([1024], BF16) for _ in range(2)]
    xT = [A.alloc([8, 512], BF16) for _ in range(2)]
    posi = A.alloc([512], I32)
    COS = A.alloc([512], F32)
    SIN = A.alloc([512], F32)
    tgk = A.alloc([512], F32)
    tgi = A.alloc([512], I32)
    qTs = [A.alloc([4, 512], BF16) for _ in range(2)]
    kThs = [A.alloc([2, 640], BF16) for _ in range(2)]
    Vhs = [A.alloc([5, 2, 65], BF16) for _ in range(2)]
    rt1 = A.alloc([512], F32)
    rt2 = A.alloc([512], F32)
    qab = A.alloc([512], BF16)
    tga, tgr = rt1, rt2
    xpre = [A.alloc([515], F32) for _ in range(2)]
    halo = A.alloc([8, 3], F32)
    cacc = [A.alloc([512], F32) for _ in range(2)]
    xacts = [A.alloc([8, 512], BF16) for _ in range(2)]
    xsB2 = [A.alloc([768], BF16) for _ in range(2)]
    zss = [[A.alloc([512], BF16) for _ in range(4)] for _ in range(2)]
    dtts = [[A.alloc([8], F32) for _ in range(4)] for _ in range(2)]
    Pb = [A.alloc([512], BF16) for _ in range(2)]
    attn = A.alloc([512], F32)
    mix = [A.alloc([1024], BF16) for _ in range(2)]
    mixT = A.alloc([8, 128], BF16)
    da = A.alloc([8], F32)
    cumtot = A.alloc([16], F32)
    ecum2 = [A.alloc([16], F32) for _ in range(2)]
    dte = A.alloc([8], F32)
    dtd = A.alloc([8], F32)
    dabc = A.alloc([8, 128], F32)
    segc = A.alloc([8, 128], F32)
    cbm = A.alloc([2, 128], F32)
    MT = A.alloc([8, 128], BF16)
    xc = A.alloc([8, 64], BF16)
    xdte2 = [A.alloc([8, 64], BF16) for _ in range(2)]
    off_y1 = A.off
    y1 = A.alloc([8, 64], F32)
    y2 = A.alloc([8, 64], F32)
    Sst = A.alloc([8, 64], F32)
    Sbf = A.alloc([8, 64], BF16)
    den = A.alloc([8], F32)
    smA = A.alloc([32], F32)

    memset("dve", halo, 0.0, ["halo%d" % c for c in range(8)])
    memset("dve", Sst, 0.0, ["Sst"])
    memset("dve", Sbf, 0.0, ["Sbf"])

    Wkv = Wo
    memT = segc.rearrange("p a b -> p (a b)").bitcast(BF16).rearrange("p (a b) -> p a b", a=8)
    memx = A.view(off_y1, [1024], F32)
    memn = MT.rearrange("p a b -> p (a b)")
    KWKV, KMEMX, KMEMN, KMEMT = ["Wo"], ["y1", "y2"], ["MT"], ["segc"]
    S.dma("pool", Wkv, xkv_d.rearrange("(c p) n -> p c n", p=128), "Wkv", writes=KWKV)
    memset("dve", Vm[:, :, :, 128:129], 1.0, ["Vm"])

    def memkv_gen():
        ssc, rsc = sm[:, 10:11], sm[:, 11:12]
        for m in range(2):
            S.dma("sp", memx, mem_d[m * 128:(m + 1) * 128, :], "memx", writes=KMEMX)
            yield
            act(junk, memx, AF.Square, KMEMX, ["junk", "mkss"], accum_out=ssc)
            yield
            act(rsc, ssc, AF.Ln, ["mkss", "epsc"], ["mkrs"], scale=1.0 / D, bias=epsc)
            yield
            act(rsc, rsc, AF.Exp, ["mkrs"], ["mkrs"], scale=-0.5)
            yield
            act(memn, memx, AF.Copy, KMEMX + ["mkrs"], KMEMN, scale=rsc)
            yield
            (pb, kb), = yield from PSa(1)
            pbb = pb.bitcast(BF16)
            tr([(pbb[:, c * 128:(c + 1) * 128], memn[:, c * 128:(c + 1) * 128]) for c in range(8)], identb,
               KMEMN + ["identb"], [kb])
            yield
            tt("dve", memT[:, :, m * 128:(m + 1) * 128], pbb.rearrange("p (c t) -> p c t", c=8),
               cols[:, C_MEMW:C_MEMW + 8].unsqueeze(2).to_broadcast([128, 8, 128]), ALU.mult,
               [kb, "cols"], KMEMT)
            PSr(kb)
            yield
        for h in range(4):
            (pb, kb), = yield from PSa(1)
            mm([(pb[:, 0:256], Wkv[:, kc, h * 128:(h + 1) * 128], memT[:, kc, :], kc == 0, kc == 7) for kc in range(8)],
               KWKV + KMEMT, [kb])
            yield
            act(KmT[:, h, :], pb[:, 0:256], AF.Copy, [kb], ["KmT"])
            PSr(kb)
            yield
        for m in range(2):
            (pb, kb), = yield from PSa(1)
            mm([(pb, memT[:, kc, m * 128:(m + 1) * 128], Wkv[:, kc, 512:1024], kc == 0, kc == 7) for kc in range(8)],
               KWKV + KMEMT, [kb])
            yield
            act(Vm[:, m, :, 0:128], pb.rearrange("p (h d) -> p h d", h=4), AF.Copy, [kb], ["Vm"])
            PSr(kb)
            yield

    chk(2)

    for q_ in range(2):
        memset("dve", Vhs[q_][:, :, :, 64:65], 1.0, ["Vh%d_%d" % (q_, c) for c in range(5)])
        memset("dve", kThs[q_], 0.0, ["kTh%d" % q_])

    def XT(s_):
        return ["xT%d_%d" % (s_ % 2, j) for j in range(4)]

    def run_streams(gens):
        gens = list(gens)
        clk = {id(g_): 0.0 for g_ in gens}
        idle_sweeps = 0
        while gens:
            progressed = False
            for g_ in sorted(gens, key=lambda x: clk[id(x)]):
                before = (tuple(S.cnt.values()), sum(S.dcnt.values()))
                try:
                    next(g_)
                except StopIteration:
                    gens.remove(g_)
                    progressed = True
                    break
                if (tuple(S.cnt.values()), sum(S.dcnt.values())) != before:
                    clk[id(g_)] = S.last_finish
                    progressed = True
                    break
            idle_sweeps = 0 if progressed else idle_sweeps + 1
            assert idle_sweeps < 10000, "stream scheduler stuck"
        assert not any(ps_busy), "PSUM bank leaked by a stream"

    def rstd_g(ss, rstd, inv_n, kss, krs):
        act(rstd, ss, AF.Ln, [kss, "epsc"], [krs], scale=inv_n, bias=epsc)
        yield
        act(rstd, rstd, AF.Exp, [krs], [krs], scale=-0.5)
        yield

    def a1_gen(s_, j0):
        xt_ = xT[s_ % 2]
        for j in (j0, j0 + 2):
            n = s_ * 4 + j
            kx = "xin%d" % j0
            kss, krs, kxn = "a1ss%d" % j0, "a1rs%d" % j0, "xn%d" % j0
            ssc = smA[:, j0 * 2:j0 * 2 + 1]
            rsc = smA[:, j0 * 2 + 1:j0 * 2 + 2]
            S.dma("sp", xin[j0], x_d[n * 128:(n + 1) * 128, :], kx, writes=[kx])
            yield
            act(junk, xin[j0], AF.Square, [kx], ["junk", kss], accum_out=ssc)
            yield
            yield from rstd_g(ssc, rsc, 1.0 / D, kss, krs)
            act(xn[j0], xin[j0], AF.Copy, [kx, krs], [kxn], scale=rsc)
            yield
            (pb, kb), = yield from PSa(1)
            pbb = pb.bitcast(BF16)
            tr([(pbb[:, c * 128:(c + 1) * 128], xn[j0][:, c * 128:(c + 1) * 128]) for c in range(8)], identb,
               [kxn, "identb"], [kb])
            yield
            tt("dve", xt_[:, :, j * 128:(j + 1) * 128], pbb.rearrange("p (c t) -> p c t", c=8),
               cols[:, C_MIXW:C_MIXW + 8].unsqueeze(2).to_broadcast([128, 8, 128]), ALU.mult,
               [kb, "cols"], [XT(s_)[j]])
            PSr(kb)
            yield

    FL = {}

    def wait(*ks):
        while not all(FL.get(k) for k in ks):
            yield

    def trig_gen(s_):
        yield from wait(("rope", s_ - 1))
        S.dma("sp", posi, pos_d[:, s_ * 512:(s_ + 1) * 512], "posi", writes=["posi"])
        cp("dve", tgk, posi, ["posi"], ["tgk"])
        yield
        ts("dve", tgk, tgk, cols[:, C_INVF:C_INVF + 1], None, ALU.mult, None, ["tgk", "cols"], ["tgk"])
        yield
        for (tab, ktab, shift) in ((SIN, "SIN", 0.0), (COS, "COS", PI / 2)):
            ts("dve", tga, tgk, shift, None, ALU.add, None, ["tgk"], ["rt1"])
            yield
            ts("dve", tgr, tga, 1.0 / (2 * PI), None, ALU.mult, None, ["rt1"], ["rt2"])
            yield
            cp("dve", tgi, tgr, ["rt2"], ["tgi"])
            yield
            cp("dve", tgr, tgi, ["tgi"], ["rt2"])
            yield
            stt("dve", tga, tgr, -2 * PI, tga, ALU.mult, ALU.add, ["rt2", "rt1"], ["rt1"])
            yield
            ts("dve", tga, tga, -PI, PI, ALU.max, ALU.min, ["rt1"], ["rt1"])
            yield
            act(tab, tga, AF.Sin, ["rt1"], [ktab])
            yield
        FL[("trig", s_)] = True

    def proj_fm(s_, c, pb, kb):
        xt_ = xT[s_ % 2]
        mm([(pb, Wf[:, kc, c * 128:(c + 1) * 128], xt_[:, kc, :], kc == 0, kc == 7) for kc in range(8)],
           ["Wf"] + XT(s_), [kb])
        return pb, kb

    def rope_gen(s_):
        q_ = s_ % 2
        qT, kTh = qTs[q_], kThs[q_]
        kqT, kkT = "qT%d" % q_, "kTh%d" % q_
        yield from wait(("trig", s_), ("swa", s_ - 2))
        if s_ > 0:
            cp("pool", kTh[:, :, 0:128], kThs[1 - q_][:, :, 512:640], ["kTh%d" % (1 - q_)], [kkT])
            yield
        items = [(c, qT[:, c, :], kqT) for c in range(4)] + \
                [(4 + g, kTh[:, g, 128:640], kkT) for g in range(2)]
        for (ca, outap, kout) in items:
            (pa, ka), (pb_, kb_) = yield from PSa(2)
            proj_fm(s_, ca, pa, ka)
            yield
            cp("act", qab, pa, [ka], ["qab"])
            yield
            mm([(pb_, rotb, qab, True, True)], ["rotb", "qab"], [kb_])
            yield
            tt("dve", rt1, pa, COS, ALU.mult, [ka, "COS"], ["rt1"])
            yield
            tt("dve", rt2, pb_, SIN, ALU.mult, [kb_, "SIN"], ["rt2"])
            PSr(ka, kb_)
            yield
            tt("pool", outap, rt1, rt2, ALU.add, ["rt1", "rt2"], [kout])
            yield
        FL[("rope", s_)] = True

    def conv_gen(s_, par):
        q_ = s_ % 2
        xact = xacts[q_]
        yield from wait(("ssd", s_ - 2))
        for c in range(par, 8, 2):
            (pa, ka), = yield from PSa(1)
            proj_fm(s_, 6 + c, pa, ka)
            yield
            xp = xpre[par]
            kxp = "xpre%d" % par
            cp("pool", xp[:, 0:3], halo[:, c, :], ["halo%d" % c], [kxp])
            yield
            act(xp[:, 3:515], pa, AF.Copy, [ka], [kxp])
            PSr(ka)
            yield
            cp("pool", halo[:, c, :], xp[:, 512:515], [kxp], ["halo%d" % c])
            yield
            ac = cacc[par]
            kac = "cacc%d" % par
            ts("dve", ac, xp[:, 0:512], cols[:, C_CONVW + c * 4:C_CONVW + c * 4 + 1], None, ALU.mult, None,
               [kxp, "cols"], [kac])
            yield
            for jt in range(1, 4):
                stt("dve", ac, xp[:, jt:jt + 512], cols[:, C_CONVW + c * 4 + jt:C_CONVW + c * 4 + jt + 1], ac,
                    ALU.mult, ALU.add, [kxp, "cols", kac], [kac])
                yield
            act(xact[:, c, :], ac, AF.Silu, [kac, "cols"], ["xact%d_%d" % (q_, c)], bias=cols[:, C_CONVB + c:C_CONVB + c + 1])
            yield
        FL[("conv", s_, par)] = True

    def a3_gen(s_):
        q_ = s_ % 2
        zs, dtt, Vh = zss[q_], dtts[q_], Vhs[q_]
        yield from wait(("swa", s_ - 2), ("ssd", s_ - 2))
        if s_ > 0:
            cp("pool", Vh[:, 0, :, :], Vhs[1 - q_][:, 4, :, :], ["Vh%d_4" % (1 - q_)], ["Vh%d_0" % q_])
            yield
        xt_ = xT[s_ % 2]
        for j in range(4):
            tok = slice(j * 128, (j + 1) * 128)
            (p1, k1), (p2, k2) = yield from PSa(2)
            mm([(p1, xt_[:, kc, tok], Wt[:, kc, 0:512], kc == 0, kc == 7) for kc in range(8)], ["Wt", XT(s_)[j]], [k1])
            yield
            act(zs[j], p1, AF.Silu, [k1], ["zs%d_%d" % (q_, j)])
            PSr(k1)
            yield
            mm([(p2[:, 0:136], xt_[:, kc, tok], Wt[:, kc, 512:648], kc == 0, kc == 7) for kc in range(8)],
               ["Wt", XT(s_)[j]], [k2])
            yield
            cp("dve", Vh[:, 1 + j, :, 0:64], p2[:, 0:128].rearrange("p (g d) -> p g d", g=2), [k2], ["Vh%d_%d" % (q_, 1 + j)])
            yield
            tt("dve", dtt[j], p2[:, 128:136], rows[:, R_DTB:R_DTB + 8], ALU.add, [k2, "rows"], ["dt%d_%d" % (q_, j)])
            PSr(k2)
            yield
        for j in range(4):
            act(dtt[j], dtt[j], AF.Exp, ["dt%d_%d" % (q_, j)], ["dt%d_%d" % (q_, j)])
            yield
            act(dtt[j], dtt[j], AF.Ln, ["dt%d_%d" % (q_, j), "ones"], ["dt%d_%d" % (q_, j)], bias=one_c)
            yield
        FL[("a3", s_)] = True

    def swa_gen(s_):
        q_ = s_ % 2
        qT, kTh, Vh = qTs[q_], kThs[q_], Vhs[q_]
        kqT, kkT = "qT%d" % q_, "kTh%d" % q_
        yield from wait(("rope", s_), ("a3", s_))
        ssc, rsc = smA[:, 8:9], smA[:, 9:10]
        for j in range(4):
            n = s_ * 4 + j
            tok = slice(j * 128, (j + 1) * 128)
            blocks = [0, 1] if n > 0 else [1]
            for g in range(2):
                pS = {}
                banks_ = yield from PSa(2 * len(blocks))
                for bi_, b in enumerate(blocks):
                    pS[b] = (banks_[2 * bi_], banks_[2 * bi_ + 1])
                    kt = slice((b + j) * 128, (b + j + 1) * 128)
                    for hf in range(2):
                        pr = slice(hf * 64, (hf + 1) * 64)
                        mm([(pS[b][hf][0][:, 0:256], identb, mbias[b].rearrange("p i q -> p (i q)"), True, False)] +
                           [(pS[b][hf][0][:, ii * 128:(ii + 1) * 128], kTh[pr, g, kt], qT[pr, 2 * g + ii, tok], False, ii == 1)
                            for ii in range(2)], [kkT, kqT, "identb", "mbias"], [pS[b][hf][1]])
                        yield
                for b in blocks:
                    kp = "P%d" % b
                    for hf in range(2):
                        act(Pb[b][:, hf * 256:(hf + 1) * 256], pS[b][hf][0][:, 0:256], AF.Exp, [pS[b][hf][1]], [kp],
                            scale=0.125)
                        PSr(pS[b][hf][1])
                        yield
                (pO, kO), = yield from PSa(1)
                lst = []
                for i in range(4):
                    for b in blocks:
                        pi = (i % 2) * 2 + i // 2
                        lst.append((pO[:, i * 65:(i + 1) * 65], Pb[b][:, pi * 128:(pi + 1) * 128], Vh[:, j + b, g, :],
                                    b == blocks[0], b == blocks[-1]))
                mm(lst, ["P0", "P1", "Vh%d_%d" % (q_, j), "Vh%d_%d" % (q_, j + 1)], [kO])
                yield
                pOv = pO[:, 0:260].rearrange("p (i d) -> p i d", i=4)
                tt("dve", den[:, 0:4], pOv[:, :, 64], esink[:, g * 4:(g + 1) * 4], ALU.add, [kO, "esink"], ["den"])
                yield
                recip(den[:, 4:8], den[:, 0:4], ["den"], ["den"])
                yield
                tt("dve", attn[:, g * 256:(g + 1) * 256].rearrange("p (i d) -> p i d", i=4), pOv[:, :, 0:64],
                   den[:, 4:8].unsqueeze(2).to_broadcast([128, 4, 64]), ALU.mult, [kO, "den"], ["attn"])
                PSr(kO)
                yield
            act(junk[:, 0:512], attn, AF.Square, ["attn"], ["junk", "swss"], accum_out=ssc)
            yield
            yield from rstd_g(ssc, rsc, 1.0 / 512, "swss", "swrs")
            yield from wait(("op", n - 2))
            stt("dve", mix[j % 2][:, 0:512], attn, rsc, rows[:, R_ANW:R_ANW + 512], ALU.mult, ALU.mult,
                ["attn", "swrs", "rows"], ["mixA%d" % (j % 2)])
            yield
            FL[("swa_c", n)] = True
        FL[("swa", s_)] = True


    PYD = {}

    def ssdF_gen(s_):
        p_ = s_ % 2
        xact, dtt = xacts[p_], dtts[p_]
        XACT = ["xact%d_%d" % (p_, c) for c in range(8)]
        yield from wait(("conv", s_, 0), ("conv", s_, 1), ("a3", s_))
        for j in range(4):
            n = s_ * 4 + j
            q_ = n % 2
            yield from wait(("ssdT", n - 2))
            tok = slice(j * 128, (j + 1) * 128)
            xsB_, kxs = xsB2[q_], "xsB%d" % q_
            xdte_, kxd = xdte2[q_], "xdte%d" % q_
            ecum_, kec = ecum2[q_], "ecum%d" % q_
            kd = "dt%d_%d" % (p_, j)
            tt("dve", da, dtt[j], abc, ALU.mult, [kd, "abc"], ["da"])
            yield
            (pc, kc_), = yield from PSa(1)
            mm([(pc[:, 0:8], trif, da, True, True), (pc[:, 8:16], ones, da, True, True)], ["trif", "ones", "da"], [kc_])
            yield
            cp("dve", cumtot, pc[:, 0:16], [kc_], ["cumtot"])
            PSr(kc_)
            yield
            pR = yield from PSa(2)
            for hb in range(2):
                mm([(pR[hb][0][:, r * 128:(r + 1) * 128], da[:, hb * 4 + r:hb * 4 + r + 1].to_broadcast([128, 128]), trif,
                     True, True) for r in range(4)], ["da", "trif"], [pR[hb][1]])
                yield
            (pb, kb), = yield from PSa(1)
            pbb = pb.bitcast(BF16)
            tr([(pbb[:, c * 128:(c + 1) * 128], xact[:, c, tok]) for c in range(6)], identb, XACT + ["identb"], [kb])
            yield
            act(ecum_, cumtot, AF.Exp, ["cumtot"], [kec])
            yield
            tt("dve", dte, cumtot[:, 8:16], cumtot[:, 0:8], ALU.subtract, ["cumtot"], ["dte"])
            yield
            act(dte, dte, AF.Exp, ["dte"], ["dte"])
            yield
            cp("act", xsB_, pbb[:, 0:768], [kb], [kxs])
            PSr(kb)
            yield
            xs3 = xsB_[:, 0:512].rearrange("p (h d) -> p h d", h=8)
            tt("dve", dtd, dtt[j], dte, ALU.mult, [kd, "dte"], ["dtd"])
            yield
            for h in range(8):
                ts("dve", segc[:, h, :], pR[h // 4][0][:, (h % 4) * 128:(h % 4 + 1) * 128], cumtot[:, h:h + 1], 0.0,
                   ALU.subtract, ALU.min, [pR[h // 4][1], "cumtot"], ["segc"])
                yield
            PSr(pR[0][1], pR[1][1])
            act(segc, segc, AF.Exp, ["segc"], ["segc"])
            yield
            (pcb, kcb), = yield from PSa(1)
            mm([(pcb[:, g * 128:(g + 1) * 128], xact[:, 4 + g, tok], xact[:, 6 + g, tok], True, True) for g in range(2)],
               XACT, [kcb])
            yield
            tt("dve", cbm, pcb[:, 0:256].rearrange("p (g l) -> p g l", g=2),
               trif.unsqueeze(1).to_broadcast([128, 2, 128]), ALU.mult, [kcb, "trif"], ["cbm"])
            PSr(kcb)
            yield
            tt("dve", xc, xs3, dtt[j].unsqueeze(2).to_broadcast([128, 8, 64]), ALU.mult, [kxs, kd], ["xc"])
            yield
            tt("pool", xdte_, xs3, dtd.unsqueeze(2).to_broadcast([128, 8, 64]), ALU.mult, [kxs, "dtd"], [kxd])
            yield
            for g in range(2):
                tt("dve", MT[:, g * 4:(g + 1) * 4, :], segc[:, g * 4:(g + 1) * 4, :],
                   cbm[:, g, :].unsqueeze(1).to_broadcast([128, 4, 128]), ALU.mult, ["segc", "cbm"], ["MT"])
                yield
            (pyd, kyd), = yield from PSa(1)
            mm([(pyd[:, h * 64:(h + 1) * 64], MT[:, h, :], xc[:, h, :], True, True) for h in range(8)],
               ["MT", "xc"], [kyd])
            PYD[n] = (pyd, kyd)
            FL[("ssdF", n)] = True
            yield

    def ssdT_gen(s_):
        p_ = s_ % 2
        xact, zs = xacts[p_], zss[p_]
        XACT = ["xact%d_%d" % (p_, c) for c in range(8)]
        for j in range(4):
            n = s_ * 4 + j
            q_ = n % 2
            yield from wait(("ssdF", n))
            tok = slice(j * 128, (j + 1) * 128)
            xsB_, kxs = xsB2[q_], "xsB%d" % q_
            xdte_, kxd = xdte2[q_], "xdte%d" % q_
            ecum_, kec = ecum2[q_], "ecum%d" % q_
            xs3 = xsB_[:, 0:512].rearrange("p (h d) -> p h d", h=8)
            pyd, kyd = PYD.pop(n)
            (pyo, kyo), (pst_, kst) = yield from PSa(2)
            mm([(pyo[:, g * 256:(g + 1) * 256], xact[:, 6 + g, tok],
                 Sbf[:, g * 4:(g + 1) * 4, :].rearrange("p h d -> p (h d)"), True, True) for g in range(2)],
               XACT + ["Sbf"], [kyo])
            yield
            mm([(pst_[:, g * 256:(g + 1) * 256], xsB_[:, 512 + g * 128:512 + (g + 1) * 128],
                 xdte_[:, g * 4:(g + 1) * 4, :].rearrange("p h d -> p (h d)"), True, True) for g in range(2)],
               [kxs, kxd], [kst])
            yield
            tt("pool", y2, xs3, rows[:, R_DSK:R_DSK + 8].unsqueeze(2).to_broadcast([128, 8, 64]), ALU.mult,
               [kxs, "rows"], ["y2"])
            yield
            tt("dve", y1, pyo.rearrange("p (h d) -> p h d", h=8), ecum_[:, 0:8].unsqueeze(2).to_broadcast([128, 8, 64]),
               ALU.mult, [kyo, kec], ["y1"])
            yield
            tt("dve", y1, y1, pyd.rearrange("p (h d) -> p h d", h=8), ALU.add, ["y1", kyd], ["y1"])
            PSr(kyo, kyd)
            yield
            tt("dve", Sst, Sst, ecum_[:, 8:16].unsqueeze(2).to_broadcast([128, 8, 64]), ALU.mult, ["Sst", kec], ["Sst"])
            yield
            tt("dve", Sst, Sst, pst_.rearrange("p (h d) -> p h d", h=8), ALU.add, ["Sst", kst], ["Sst"])
            PSr(kst)
            yield
            cp("act", Sbf, Sst, ["Sst"], ["Sbf"])
            yield
            tt("dve", y1, y1, y2, ALU.add, ["y1", "y2"], ["y1"])
            yield
            y1f = y1.rearrange("p h d -> p (h d)")
            tt("dve", y1f, y1f, zs[j], ALU.mult, ["y1", "zs%d_%d" % (p_, j)], ["y1"])
            yield
            for g in range(2):
                act(junk[:, 0:256], y1f[:, g * 256:(g + 1) * 256], AF.Square, ["y1"], ["junk", "sdss"],
                    accum_out=smA[:, 12 + g:13 + g])
                yield
            yield from rstd_g(smA[:, 12:14], smA[:, 14:16], 1.0 / 256, "sdss", "sdrs")
            yield from wait(("op", n - 2))
            for g in range(2):
                stt("dve", mix[j % 2][:, 512 + g * 256:512 + (g + 1) * 256], y1f[:, g * 256:(g + 1) * 256],
                    smA[:, 14 + g:15 + g], rows[:, R_SNW + g * 256:R_SNW + (g + 1) * 256], ALU.mult, ALU.mult,
                    ["y1", "sdrs", "rows"], ["mixB%d" % (j % 2)])
                yield
            FL[("ssd_c", n)] = True
            FL[("ssdT", n)] = True
        FL[("ssd", s_)] = True

    def op_gen(s_):
        for j in range(4):
            n = s_ * 4 + j
            kx = "xrs0"
            yield from wait(("swa_c", n), ("ssd_c", n))
            S.dma("sp", xrs[0], x_d[n * 128:(n + 1) * 128, :], kx, writes=[kx])
            (pb, kb), = yield from PSa(1)
            pbb = pb.bitcast(BF16)
            mx_ = mix[j % 2]
            tr([(pbb[:, c * 128:(c + 1) * 128], mx_[:, c * 128:(c + 1) * 128]) for c in range(8)], identb,
               ["mixA%d" % (j % 2), "mixB%d" % (j % 2), "identb"], [kb])
            FL[("op", n)] = True
            yield
            cp("act", mixT.rearrange("p c t -> p (c t)"), pbb, [kb], ["mixT"])
            PSr(kb)
            yield
            for hf in range(2):
                (po, ko), = yield from PSa(1)
                mm([(po, mixT[:, kc, :], Wo[:, kc, hf * 512:(hf + 1) * 512], kc == 0, kc == 7) for kc in range(8)],
                   ["mixT", "Wo"], [ko])
                yield
                tt("dve", xrs[0][:, hf * 512:(hf + 1) * 512], po, xrs[0][:, hf * 512:(hf + 1) * 512], ALU.add,
                   [ko, kx], [kx])
                PSr(ko)
                yield
            S.dma("sp", x2_d[n * 128:(n + 1) * 128, :], xrs[0], kx, reads=[kx], writes=["x2d%d" % n])
            yield

    for k_ in ("swa", "ssd", "op", "ssdT", "rope"):
        FL[(k_, -1)] = True
        FL[(k_, -2)] = True
    run_streams([a1_gen(0, 0), a1_gen(0, 1), trig_gen(0)])
    g0 = [rope_gen(0), conv_gen(0, 0), conv_gen(0, 1), a3_gen(0), memkv_gen()]
    if NST > 1:
        g0 += [a1_gen(1, 0), a1_gen(1, 1), trig_gen(1)]
    run_streams(g0)
    S.dma("pool", Wo, wo_d.rearrange("(c p) n -> p c n", p=128), "Wo", writes=["Wo"])
    for st in range(NST):
        gens = [swa_gen(st), ssdF_gen(st), ssdT_gen(st), op_gen(st)]
        if st + 1 < NST:
            gens += [rope_gen(st + 1), conv_gen(st + 1, 0), conv_gen(st + 1, 1), a3_gen(st + 1)]
        if st + 2 < NST:
            gens += [a1_gen(st + 2, 0), a1_gen(st + 2, 1), trig_gen(st + 2)]
        run_streams(gens)
        chk(8)

    chk(9)
    A_keys = list(S.last_w.keys())
    A.off = mark0
    S.fence(A_keys)

    yacc = A.alloc([NPC, 1024], F32)
    x3nT = A.alloc([8, TP], BF16)
    gates = A.alloc([NPC, 32], F32)
    rstdm = A.alloc([NPC], F32)
    outb = [A.alloc([1024], F32) for _ in range(2)]
    smB = A.alloc([32], F32)
    Wxq = A.alloc([8, 512], BF16)
    Wxo = A.alloc([4, 1024], BF16)
    Wg0 = A.alloc([8, 256], BF16)
    Wu0 = A.alloc([8, 256], BF16)
    Wd0 = A.alloc([2, 1024], BF16)
    mark1 = A.off
    S.dma("pool", Wxq, xq_d.rearrange("(c p) n -> p c n", p=128), "Wxq", writes=["Wxq"])
    S.dma("pool", Wxo, xo_d.rearrange("(c p) n -> p c n", p=128), "Wxo", writes=["Wxo"])

    def load_w0():
        S.dma("pool", Wg0, wg_d[0].rearrange("(c p) f -> p c f", p=128), "We0", writes=["We0"])
        S.dma("pool", Wu0, wu_d[0].rearrange("(c p) f -> p c f", p=128), "We0", writes=["We0"])
        S.dma("pool", Wd0, wd_d[0].rearrange("(c p) n -> p c n", p=128), "We0", writes=["We0"])
    NSTP = NPC // 4

    FN = {}

    def fn_gen(pp):
        ssc, rsc = smB[:, 16:17], smB[:, 17:18]
        for ci in range(NPC):
            n = pp * NPC + ci
            ky = "y%d" % ci
            act(junk, yacc[:, ci, :], AF.Square, [ky], ["junk", "fnss"], accum_out=ssc)
            yield
            yield from rstd_g(ssc, rsc, 1.0 / D, "fnss", "fnrs")
            kob = "outb%d" % (ci % 2)
            stt("dve", outb[ci % 2], yacc[:, ci, :], rsc, rows[:, R_FNW:R_FNW + 1024], ALU.mult, ALU.mult,
                [ky, "fnrs", "rows"], [kob])
            FN[(pp, ci)] = True
            yield
            S.dma("sp", out_d[n * 128:(n + 1) * 128, :], outb[ci % 2], kob, reads=[kob])
            yield

    for p in range(NPASS):
        A.off = mark1
        if p == 0:
            S.fence(list(S.last_w.keys()))
        else:
            S.fence(["We1", "sg0", "sg1", "hid0", "hid1"])
        Wr32 = A.alloc([8, 36], F32)
        x2n = [A.alloc([1024], BF16) for _ in range(2)]
        x3n = [A.alloc([1024], BF16) for _ in range(2)]
        x2nT = A.alloc([8, 512], BF16)
        qxT = A.alloc([4, 512], BF16)
        PxT = [A.alloc([512], BF16) for _ in range(4)]
        ob = [A.alloc([4, 512], BF16) for _ in range(2)]
        oT = [A.alloc([4, 128], BF16) for _ in range(2)]
        x3T32 = [A.alloc([8, 128], F32) for _ in range(2)]
        lgs = [A.alloc([36], F32) for _ in range(2)]
        rsms = [A.alloc([32], F32) for _ in range(2)]
        msks = [A.alloc([32], F32) for _ in range(2)]
        g1s = [A.alloc([32], F32) for _ in range(2)]
        top8s = [A.alloc([8], F32) for _ in range(2)]
        load_w0()
        S.dma("sp", Wr32, wr_d.rearrange("(c p) n -> p c n", p=128), "Wr32", writes=["Wr32"])
        tt("dve", Wr32, Wr32, cols[:, C_FFNW:C_FFNW + 8].unsqueeze(2).to_broadcast([128, 8, 36]), ALU.mult,
           ["Wr32", "cols"], ["Wr32"])
        FB = {}

        def waitb(*ks):
            while not all(FB.get(k) for k in ks):
                yield

        def la_gen(par, p=p):
            ssc, rsc = smB[:, par * 2:par * 2 + 1], smB[:, par * 2 + 1:par * 2 + 2]
            kss, krs, kxn = "lass%d" % par, "lars%d" % par, "x2n%d" % par
            for s_ in range(NSTP):
                for j in (par, par + 2):
                    ci = s_ * 4 + j
                    n = p * NPC + ci
                    ky = "y%d" % ci
                    while p > 0 and not FN.get((p - 1, ci)):
                        yield
                    S.dma("sp", yacc[:, ci, :], x2_d[n * 128:(n + 1) * 128, :], ky, reads=["x2d%d" % n], writes=[ky])
                    yield
                    act(junk, yacc[:, ci, :], AF.Square, [ky], ["junk", kss], accum_out=ssc)
                    yield
                    yield from rstd_g(ssc, rsc, 1.0 / D, kss, krs)
                    act(x2n[par], yacc[:, ci, :], AF.Copy, [ky, krs], [kxn], scale=rsc)
                    yield
                    yield from waitb(("qx", s_ - 1))
                    (pb, kb), = yield from PSa(1)
                    pbb = pb.bitcast(BF16)
                    tr([(pbb[:, c * 128:(c + 1) * 128], x2n[par][:, c * 128:(c + 1) * 128]) for c in range(8)], identb,
                       [kxn, "identb"], [kb])
                    yield
                    tt("dve", x2nT[:, :, j * 128:(j + 1) * 128], pbb.rearrange("p (c t) -> p c t", c=8),
                       cols[:, C_XAW:C_XAW + 8].unsqueeze(2).to_broadcast([128, 8, 128]), ALU.mult,
                       [kb, "cols"], ["x2nT%d" % j])
                    PSr(kb)
                    yield
                FB[("la", s_, par)] = True

        X2 = ["x2nT%d" % j for j in range(4)]

        def xa_gen():
            for s_ in range(NSTP):
                yield from waitb(("la", s_, 0), ("la", s_, 1))
                for h in range(4):
                    (pb, kb), = yield from PSa(1)
                    mm([(pb, Wxq[:, kc, h * 128:(h + 1) * 128], x2nT[:, kc, :], kc == 0, kc == 7) for kc in range(8)],
                       ["Wxq"] + X2, [kb])
                    yield
                    act(qxT[:, h, :], pb, AF.Copy, [kb], ["qxT"])
                    PSr(kb)
                    yield
                FB[("qx", s_)] = True
                yield from waitb(("xo", s_ - 2, 0), ("xo", s_ - 2, 1))
                obs = ob[s_ % 2]
                for h in range(4):
                    for m in range(2):
                        (pb, kb), = yield from PSa(1)
                        mm([(pb, KmT[:, h, m * 128:(m + 1) * 128], qxT[:, h, :], True, True)], ["KmT", "qxT"], [kb])
                        yield
                        act(PxT[(h % 2) * 2 + m], pb, AF.Exp, [kb], ["PxT%d" % ((h % 2) * 2 + m)], scale=float(128 ** -0.5))
                        PSr(kb)
                        yield
                    for j in range(4):
                        (pO, kO), = yield from PSa(1)
                        mm([(pO[:, 0:129], PxT[(h % 2) * 2 + m][:, j * 128:(j + 1) * 128], Vm[:, m, h, :], m == 0, m == 1)
                            for m in range(2)], ["PxT%d" % ((h % 2) * 2), "PxT%d" % ((h % 2) * 2 + 1), "Vm"], [kO])
                        yield
                        recip(smB[:, 8:9], pO[:, 128:129], [kO], ["rd"])
                        yield
                        ts("dve", obs[:, j, h * 128:(h + 1) * 128], pO[:, 0:128], smB[:, 8:9], None, ALU.mult, None,
                           [kO, "rd"], ["ob%d_%d" % (s_ % 2, j)])
                        PSr(kO)
                        yield
                FB[("xa", s_)] = True

        def xo_gen(par):
            ssc = smB[:, 12 + par:13 + par]
            kss = "xoss%d" % par
            lg, rsm, msk, g1, top8 = lgs[par], rsms[par], msks[par], g1s[par], top8s[par]
            klg, krsm, kmsk, kg1, kt8 = "lg%d" % par, "rsm%d" % par, "msk%d" % par, "g1%d" % par, "t8%d" % par
            for s_ in range(NSTP):
                yield from waitb(("xa", s_))
                obs = ob[s_ % 2]
                for j in (par, par + 2):
                    ci = s_ * 4 + j
                    ky = "y%d" % ci
                    kob = "ob%d_%d" % (s_ % 2, j)
                    koT, kx3, k32 = "oT%d" % par, "x3n%d" % par, "x3T%d" % par
                    (pb, kb), = yield from PSa(1)
                    pbb = pb.bitcast(BF16)
                    tr([(pbb[:, c * 128:(c + 1) * 128], obs[:, j, c * 128:(c + 1) * 128]) for c in range(4)], identb,
                       [kob, "identb"], [kb])
                    yield
                    cp("act", oT[par].rearrange("p c t -> p (c t)"), pbb[:, 0:512], [kb], [koT])
                    PSr(kb)
                    yield
                    for hf in range(2):
                        (po, ko), = yield from PSa(1)
                        mm([(po, oT[par][:, kc, :], Wxo[:, kc, hf * 512:(hf + 1) * 512], kc == 0, kc == 3)
                            for kc in range(4)], [koT, "Wxo"], [ko])
                        yield
                        tt("dve", yacc[:, ci, hf * 512:(hf + 1) * 512], po, yacc[:, ci, hf * 512:(hf + 1) * 512], ALU.add,
                           [ko, ky], [ky])
                        PSr(ko)
                        yield
                    act(junk, yacc[:, ci, :], AF.Square, [ky], ["junk", kss], accum_out=ssc)
                    yield
                    yield from rstd_g(ssc, rstdm[:, ci:ci + 1], 1.0 / D, kss, "rstdm%d" % ci)
                    krm = "rstdm%d" % ci
                    act(x3n[par], yacc[:, ci, :], AF.Copy, [ky, krm], [kx3], scale=rstdm[:, ci:ci + 1])
                    yield
                    (pb, kb), = yield from PSa(1)
                    pbb = pb.bitcast(BF16)
                    tr([(pbb[:, c * 128:(c + 1) * 128], x3n[par][:, c * 128:(c + 1) * 128]) for c in range(8)], identb,
                       [kx3, "identb"], [kb])
                    yield
                    tt("dve", x3nT[:, :, ci * 128:(ci + 1) * 128], pbb.rearrange("p (c t) -> p c t", c=8),
                       cols[:, C_FFNW:C_FFNW + 8].unsqueeze(2).to_broadcast([128, 8, 128]), ALU.mult,
                       [kb, "cols"], ["x3nT%d" % (ci // 4)])
                    PSr(kb)
                    yield
                    for hb in range(2):
                        (pb, kb), = yield from PSa(1)
                        tr([(pb[:, c * 128:(c + 1) * 128], yacc[:, ci, (hb * 4 + c) * 128:(hb * 4 + c + 1) * 128])
                            for c in range(4)], identf, [ky, "identf"], [kb])
                        yield
                        act(x3T32[par][:, hb * 4:(hb + 1) * 4, :].rearrange("p c t -> p (c t)"), pb, AF.Copy, [kb], [k32])
                        PSr(kb)
                        yield
                    (pl, kl), = yield from PSa(1)
                    mm([(pl[:, 0:36], x3T32[par][:, kc, :], Wr32[:, kc, :], kc == 0, kc == 7) for kc in range(8)],
                       [k32, "Wr32"], [kl])
                    yield
                    stt("dve", lg, pl[:, 0:36], rstdm[:, ci:ci + 1], rows[:, R_RB:R_RB + 36], ALU.mult, ALU.add,
                        [kl, krm, "rows"], [klg])
                    PSr(kl)
                    yield
                    S.op("dve", lambda e: e.tensor_reduce(out=rsm[:, 0:1], in_=lg[:, 0:4], axis=mybir.AxisListType.X,
                                                          op=ALU.max), [klg], [krsm])
                    yield
                    ts("dve", rsm[:, 1:2], rsm[:, 0:1], -1.0, None, ALU.mult, None, [krsm], [krsm])
                    yield
                    act(rsm[:, 4:8], lg[:, 0:4], AF.Exp, [klg, krsm], [krsm], bias=rsm[:, 1:2], accum_out=rsm[:, 2:3])
                    yield
                    recip(rsm[:, 3:4], rsm[:, 2:3], [krsm], [krsm])
                    yield
                    ts("dve", rsm[:, 8:12], lg[:, 0:4], rsm[:, 0:1], 1e30, ALU.is_ge, ALU.mult, [klg, krsm], [krsm])
                    yield
                    ts("dve", rsm[:, 8:12], rsm[:, 8:12], -1e30, None, ALU.add, None, [krsm], [krsm])
                    yield
                    tt("dve", msk.rearrange("p (g e) -> p g e", g=4), lg[:, 4:36].rearrange("p (g e) -> p g e", g=4),
                       rsm[:, 8:12].unsqueeze(2).to_broadcast([128, 4, 8]), ALU.add, [klg, krsm], [kmsk])
                    yield
                    S.op("dve", lambda e: e.max(out=top8, in_=msk), [kmsk], [kt8])
                    yield
                    tt("dve", rsm[:, 12:13], top8[:, 1:2], top8[:, 0:1], ALU.subtract, [kt8], [krsm])
                    yield
                    act(rsm[:, 12:13], rsm[:, 12:13], AF.Exp, [krsm], [krsm])
                    yield
                    ts("dve", rsm[:, 12:13], rsm[:, 12:13], 1.0, None, ALU.add, None, [krsm], [krsm])
                    yield
                    recip(rsm[:, 13:14], rsm[:, 12:13], [krsm], [krsm])
                    yield
                    tt("dve", rsm[:, 14:15], rsm[:, 13:14], rsm[:, 3:4], ALU.mult, [krsm], [krsm])
                    yield
                    tt("dve", rsm[:, 15:16], rsm[:, 3:4], rsm[:, 14:15], ALU.subtract, [krsm], [krsm])
                    yield
                    ts("dve", g1, msk, top8[:, 0:1], rsm[:, 14:15], ALU.is_equal, ALU.mult, [kmsk, kt8, krsm], [kg1])
                    yield
                    ts("dve", msk, msk, top8[:, 1:2], rsm[:, 15:16], ALU.is_equal, ALU.mult, [kmsk, kt8, krsm], [kmsk])
                    yield
                    tt("dve", gates[:, ci, :], g1, msk, ALU.add, [kg1, kmsk], ["gates"])
                    yield
                FB[("xo", s_, par)] = True

        FB[("qx", -1)] = True
        for q_ in (-1, -2):
            FB[("xo", q_, 0)] = True
            FB[("xo", q_, 1)] = True
        run_streams([la_gen(0), la_gen(1), xa_gen(), xo_gen(0), xo_gen(1)] + ([fn_gen(p - 1)] if p > 0 else []))
        chk(11)

        A.off = mark1
        S.fence(list(S.last_w.keys()))
        Wg = [Wg0, A.alloc([8, 256], BF16)]
        Wu = [Wu0, A.alloc([8, 256], BF16)]
        Wd = [Wd0, A.alloc([2, 1024], BF16)]
        sg = [A.alloc([512], F32) for _ in range(2)]
        hid = [A.alloc([2, 512], BF16) for _ in range(2)]
        hot = 0
        X3 = ["x3nT%d" % i for i in range(NPC // 4)]
        steps = [(e_, st) for e_ in range(32) for st in range(NSTP)]
        gu_i = [0]

        def PSg():
            i = gu_i[0] % 2
            gu_i[0] += 1
            return ((pst[:, (2 * i) * 512:(2 * i + 1) * 512], "ps%d" % (2 * i)),
                    (pst[:, (2 * i + 1) * 512:(2 * i + 2) * 512], "ps%d" % (2 * i + 1)))
        dn_i = [0]

        def PSd():
            i = 4 + dn_i[0] % 4
            dn_i[0] += 1
            return pst[:, i * 512:(i + 1) * 512], "ps%d" % i

        def load_w(e_):
            sl = e_ % 2
            kw = "We%d" % sl
            S.dma("pool", Wg[sl], wg_d[e_].rearrange("(c p) f -> p c f", p=128), kw, writes=[kw])
            S.dma("pool", Wu[sl], wu_d[e_].rearrange("(c p) f -> p c f", p=128), kw, writes=[kw])
            S.dma("pool", Wd[sl], wd_d[e_].rearrange("(c p) n -> p c n", p=128), kw, writes=[kw])

        def gu(step, f):
            e_, st = step
            sl = e_ % 2
            kw = "We%d" % sl
            stok = slice(st * 512, (st + 1) * 512)
            (pg_, kg_), (pu_, ku_) = PSg()
            mm([(pg_, Wg[sl][:, kc, f * 128:(f + 1) * 128], x3nT[:, kc, stok], kc == 0, kc == 7)
                for kc in range(8)], [kw, X3[st]], [kg_])
            mm([(pu_, Wu[sl][:, kc, f * 128:(f + 1) * 128], x3nT[:, kc, stok], kc == 0, kc == 7)
                for kc in range(8)], [kw, X3[st]], [ku_])
            return (pg_, kg_, pu_, ku_)

        pend = {0: gu(steps[0], 0)}
        for k, step in enumerate(steps):
            e_, st = step
            sl = e_ % 2
            kw = "We%d" % sl
            pend[1] = gu(step, 1)
            hh = hid[hot]
            kh = "hid%d" % hot
            hot ^= 1
            for f in range(2):
                pg_, kg_, pu_, ku_ = pend[f]
                act(sg[f], pg_, AF.Silu, [kg_], ["sg%d" % f])
                tt("dve", hh[:, f, :], sg[f], pu_, ALU.mult, ["sg%d" % f, ku_], [kh])
            if k + 1 < len(steps):
                if steps[k + 1][0] != e_:
                    load_w(steps[k + 1][0])
                pend[0] = gu(steps[k + 1], 0)
            for j in range(4):
                ci = st * 4 + j
                ky = "y%d" % ci
                for hf in range(2):
                    po, ko = PSd()
                    mm([(po, hh[:, f, j * 128:(j + 1) * 128], Wd[sl][:, f, hf * 512:(hf + 1) * 512], f == 0, f == 1)
                        for f in range(2)], [kh, kw], [ko])
                    stt("dve", yacc[:, ci, hf * 512:(hf + 1) * 512], po, gates[:, ci, e_:e_ + 1],
                        yacc[:, ci, hf * 512:(hf + 1) * 512], ALU.mult, ALU.add, [ko, "gates", ky], [ky])
        chk(12)
    run_streams([fn_gen(NPASS - 1)])
    S.finish("sp")
    return nc


def _prep_shared(inp):
    f32 = np.float32
    w_in = np.asarray(inp["w_in"], f32)[0]
    q = w_in[:, 0:512]
    k = w_in[:, 512:640]
    v = w_in[:, 640:768]
    z = w_in[:, 768:1280]
    xbc = w_in[:, 1280:2304]
    dt = w_in[:, 2304:2312]

    def rot(wc, nh):
        w3 = wc.reshape(D, nh, 64).copy()
        r = w3.copy()
        r[:, :, 0:8] = w3[:, :, 8:16]
        r[:, :, 8:16] = w3[:, :, 0:8]
        return r.reshape(D, nh * 64)

    qr = rot(q, 8)
    kr = rot(k, 2)
    kd = [np.concatenate([k[:, g * 64:(g + 1) * 64]] * 2, axis=1) for g in range(2)]
    krd = [np.concatenate([kr[:, g * 64:(g + 1) * 64]] * 2, axis=1) for g in range(2)]
    wf = np.ascontiguousarray(np.concatenate([q, kd[0], kd[1], xbc], axis=1))
    rotm = np.zeros((128, 128), f32)
    for m_ in range(128):
        d_ = m_ % 64
        k_ = m_ + 8 if d_ < 8 else (m_ - 8 if d_ < 16 else m_)
        rotm[k_, m_] = 1.0
    wt = np.ascontiguousarray(np.concatenate([z, v, dt], axis=1))
    assert wf.shape == (D, 1792) and wt.shape == (D, 648)

    def colv(vv):
        return np.asarray(vv, f32).reshape(8, 128).T

    cols = np.zeros((128, NCOL), f32)
    cols[:, C_MIXW:C_MIXW + 8] = colv(inp["mix_norm_w"][0])
    cols[:, C_XAW:C_XAW + 8] = colv(inp["xattn_norm_w"][0])
    cols[:, C_MEMW:C_MEMW + 8] = colv(inp["mem_norm_w"][0])
    cols[:, C_FFNW:C_FFNW + 8] = colv(inp["ffn_norm_w"][0])
    cols[:, C_CONVB:C_CONVB + 8] = colv(inp["ssd_conv_b"][0])
    cw = np.asarray(inp["ssd_conv_w"], f32)[0]
    for c in range(8):
        for j in range(4):
            cols[:, C_CONVW + c * 4 + j] = cw[j, c * 128:(c + 1) * 128]
    inv = (np.float32(500000.0) ** (-np.arange(0, 16, 2, dtype=np.float32) / np.float32(16))).astype(f32)
    invf = np.zeros(128, f32)
    for p_ in range(128):
        d_ = p_ % 64
        if d_ < 8:
            invf[p_] = -inv[d_]
        elif d_ < 16:
            invf[p_] = inv[d_ - 8]
    cols[:, C_INVF] = invf

    rows1 = np.zeros(NROW, f32)
    rows1[R_SINK:R_SINK + 8] = inp["attn_sinks"][0]
    rows1[R_DTB:R_DTB + 8] = inp["ssd_dt_bias"][0]
    rows1[R_ALOG:R_ALOG + 8] = inp["ssd_a_log"][0]
    rows1[R_DSK:R_DSK + 8] = inp["ssd_d"][0]
    rows1[R_RB:R_RB + 4] = inp["router_group_b"][0]
    rows1[R_RB + 4:R_RB + 36] = inp["router_expert_b"][0]
    rows1[R_ANW:R_ANW + 512] = inp["attn_out_norm_w"][0]
    rows1[R_SNW:R_SNW + 512] = inp["ssd_out_norm_w"][0]
    rows1[R_FNW:R_FNW + 1024] = inp["final_norm_w"]
    rows = np.ascontiguousarray(np.broadcast_to(rows1, (128, NROW)))
    wr = np.ascontiguousarray(np.concatenate([np.asarray(inp["router_group_w"], f32)[0],
                                              np.asarray(inp["router_expert_w"], f32)[0]], axis=1))
    return {
        "wf": wf, "wt": wt, "wo": np.ascontiguousarray(np.asarray(inp["w_out"], f32)[0]),
        "xq": np.ascontiguousarray(np.asarray(inp["xattn_w_q"], f32)[0]),
        "xkv": np.ascontiguousarray(np.asarray(inp["xattn_w_kv"], f32)[0]),
        "xo": np.ascontiguousarray(np.asarray(inp["xattn_w_o"], f32)[0]),
        "wr": wr,
        "wg": np.ascontiguousarray(np.asarray(inp["expert_w_gate"], f32)[0]),
        "wu": np.ascontiguousarray(np.asarray(inp["expert_w_up"], f32)[0]),
        "wd": np.ascontiguousarray(np.asarray(inp["expert_w_down"], f32)[0]),
        "cols": cols, "rows": rows,
        "ident": np.eye(128, dtype=f32), "tri": np.triu(np.ones((128, 128), f32)), "rotm": rotm,
    }


def run(inp, T, stop=99):
    shared = _prep_shared(inp)
    nc = build(T, stop)
    in_maps = []
    for b in range(NCORES):
        m = dict(shared)
        m["x"] = np.ascontiguousarray(np.asarray(inp["x"], np.float32)[b, :T])
        m["mem"] = np.ascontiguousarray(np.asarray(inp["mem"], np.float32)[b])
        m["pos"] = np.ascontiguousarray(np.broadcast_to(np.asarray(inp["positions"], np.int32)[b, :T][None, :], (128, T)))
        in_maps.append(m)
    res = run_bass_kernel_spmd(nc, in_maps, core_ids=list(range(NCORES)))
    return np.stack([np.asarray(r["out"], np.float32) for r in res.results], axis=0)


def kernel(**inputs):
    return run(inputs, SEQ)
```
